# Optimizing a Trainium2 kernel written in Bass

```python
import math
import jax, jax.numpy as jnp
from jax import lax
import numpy as np

D_MODEL = 1024
BATCH = 16
SEQ = 4096
DEPTH = 1

MIX_WIDTH = D_MODEL
ATTN_WIDTH = MIX_WIDTH // 2
CONV_WIDTH = MIX_WIDTH - ATTN_WIDTH
HEAD_DIM = 64
N_HEADS = ATTN_WIDTH // HEAD_DIM
IN_COLS = 3 * ATTN_WIDTH + 2 * CONV_WIDTH
CONV_K = 31
BLOCK = 256
TOP_K = 3
Q_CHUNK = 32
D_FF = 2816
ALIBI_MAX = 8.0
EPS = 1e-6
NEG = -1e30

kernel_name = "hybrid_conv_moba_macaron"


def _rmsnorm(x, g):
    xf = x.astype(jnp.float32)
    y = xf * lax.rsqrt(jnp.mean(xf * xf, axis=-1, keepdims=True) + EPS)
    return (y * g.astype(jnp.float32)).astype(x.dtype)


def _layernorm(x, g, b):
    xf = x.astype(jnp.float32)
    mu = jnp.mean(xf, axis=-1, keepdims=True)
    var = jnp.mean(jnp.square(xf - mu), axis=-1, keepdims=True)
    y = (xf - mu) * lax.rsqrt(var + EPS)
    return (y * g.astype(jnp.float32) + b.astype(jnp.float32)).astype(x.dtype)


def _swiglu(h, w1, w3, w2):
    return (jax.nn.silu(h @ w1) * (h @ w3)) @ w2


def _alibi_slopes(n_heads):
    return jnp.exp2(-ALIBI_MAX * (jnp.arange(n_heads, dtype=jnp.float32) + 1.0) / n_heads)


def _conv_module(a, g, dw_w, dw_b, ln_g, ln_b):
    u = a * jax.nn.sigmoid(g)
    c = u.shape[-1]
    rhs = dw_w.astype(u.dtype)[:, None, :]
    y = lax.conv_general_dilated(u, rhs, window_strides=(1,),
                                 padding=[(CONV_K - 1, 0)],
                                 dimension_numbers=("NWC", "WIO", "NWC"),
                                 feature_group_count=c)
    y = y + dw_b.astype(u.dtype)
    y = _layernorm(y, ln_g, ln_b)
    return jax.nn.silu(y)


def _moba_attention(q, k, v, slopes):
    B, H, S, hd = q.shape
    nb = -(-S // BLOCK)
    pad = nb * BLOCK - S
    kp = jnp.pad(k, ((0, 0), (0, 0), (0, pad), (0, 0)))
    vp = jnp.pad(v, ((0, 0), (0, 0), (0, pad), (0, 0)))
    kb = kp.reshape(B, H, nb, BLOCK, hd)
    vb = vp.reshape(B, H, nb, BLOCK, hd)
    kmean = jnp.mean(kb.astype(jnp.float32), axis=3)

    pos = jnp.arange(S)
    qblk = pos // BLOCK
    gate = jnp.einsum("bhsd,bhnd->bhsn", q.astype(jnp.float32), kmean)
    fully_past = jnp.arange(nb)[None, :] < qblk[:, None]
    gate = jnp.where(fully_past, gate, NEG)
    kk = min(TOP_K, nb)
    _, sel = lax.top_k(gate, kk)
    sel_valid = jnp.arange(kk)[None, :] < qblk[:, None]

    scale = hd ** -0.5
    bi = jnp.arange(B)[:, None, None]
    hi = jnp.arange(H)[None, :, None]
    blk_off = jnp.arange(BLOCK)

    def chunk(ci):
        start = ci * Q_CHUNK
        qc = lax.dynamic_slice_in_dim(q, start, Q_CHUNK, axis=2)
        selc = lax.dynamic_slice_in_dim(sel, start, Q_CHUNK, axis=2)
        validc = lax.dynamic_slice_in_dim(sel_valid, start, Q_CHUNK, axis=0)
        qpos = start + jnp.arange(Q_CHUNK)
        flat = selc.reshape(B, H, Q_CHUNK * kk)
        kg = kb[bi, hi, flat].reshape(B, H, Q_CHUNK, kk, BLOCK, hd)
        vg = vb[bi, hi, flat].reshape(B, H, Q_CHUNK, kk, BLOCK, hd)
        kpos_sel = selc[..., None] * BLOCK + blk_off
        dist_p = (qpos[:, None, None] - kpos_sel).astype(jnp.float32)
        lp = (jnp.einsum("bhcd,bhckld->bhckl", qc, kg).astype(jnp.float32) * scale
              - slopes[:, None, None, None] * jnp.abs(dist_p))
        lp = jnp.where(validc[None, None, :, :, None], lp, NEG)
        own_start = (start // BLOCK) * BLOCK
        ko = lax.dynamic_slice_in_dim(kp, own_start, BLOCK, axis=2)
        vo = lax.dynamic_slice_in_dim(vp, own_start, BLOCK, axis=2)
        kpos_own = own_start + blk_off
        dist_o = (qpos[:, None] - kpos_own[None, :]).astype(jnp.float32)
        lo = (jnp.einsum("bhcd,bhld->bhcl", qc, ko).astype(jnp.float32) * scale
              - slopes[:, None, None] * jnp.abs(dist_o))
        lo = jnp.where(kpos_own[None, :] <= qpos[:, None], lo, NEG)
        logits = jnp.concatenate([lp.reshape(B, H, Q_CHUNK, kk * BLOCK), lo], axis=-1)
        p = jax.nn.softmax(logits, axis=-1).astype(v.dtype)
        pp = p[..., :kk * BLOCK].reshape(B, H, Q_CHUNK, kk, BLOCK)
        po = p[..., kk * BLOCK:]
        return (jnp.einsum("bhckl,bhckld->bhcd", pp, vg)
                + jnp.einsum("bhcl,bhld->bhcd", po, vo))

    outs = lax.map(chunk, jnp.arange(S // Q_CHUNK))
    return outs.transpose(1, 2, 0, 3, 4).reshape(B, H, S, hd)


def setup_inputs(seed: int = 0) -> dict:
    key = jax.random.key(seed)
    ks = jax.random.split(key, 20)
    f32 = jnp.float32

    def nrm(k, shape, fan_in):
        return jax.random.normal(k, shape, f32) * fan_in ** -0.5

    def gain(k, shape):
        return 1.0 + 0.02 * jax.random.normal(k, shape, f32)

    L = DEPTH
    return {
        "x": jax.random.normal(ks[0], (BATCH, SEQ, D_MODEL), f32),
        "ffn1_norm": gain(ks[1], (L, D_MODEL)),
        "ffn1_w1": nrm(ks[2], (L, D_MODEL, D_FF), D_MODEL),
        "ffn1_w3": nrm(ks[3], (L, D_MODEL, D_FF), D_MODEL),
        "ffn1_w2": nrm(ks[4], (L, D_FF, D_MODEL), D_FF),
        "mix_norm": gain(ks[5], (L, D_MODEL)),
        "w_in": nrm(ks[6], (L, D_MODEL, IN_COLS), D_MODEL),
        "q_norm": gain(ks[7], (L, HEAD_DIM)),
        "k_norm": gain(ks[8], (L, HEAD_DIM)),
        "conv_dw_w": nrm(ks[9], (L, CONV_K, CONV_WIDTH), CONV_K),
        "conv_dw_b": 0.02 * jax.random.normal(ks[10], (L, CONV_WIDTH), f32),
        "conv_ln_g": gain(ks[11], (L, CONV_WIDTH)),
        "conv_ln_b": 0.02 * jax.random.normal(ks[12], (L, CONV_WIDTH), f32),
        "w_out": nrm(ks[13], (L, MIX_WIDTH, D_MODEL), MIX_WIDTH),
        "ffn2_norm": gain(ks[14], (L, D_MODEL)),
        "ffn2_w1": nrm(ks[15], (L, D_MODEL, D_FF), D_MODEL),
        "ffn2_w3": nrm(ks[16], (L, D_MODEL, D_FF), D_MODEL),
        "ffn2_w2": nrm(ks[17], (L, D_FF, D_MODEL), D_FF),
    }


def reference(x, ffn1_norm, ffn1_w1, ffn1_w3, ffn1_w2, mix_norm, w_in, q_norm,
              k_norm, conv_dw_w, conv_dw_b, conv_ln_g, conv_ln_b, w_out,
              ffn2_norm, ffn2_w1, ffn2_w3, ffn2_w2):
    B, S, _ = x.shape
    slopes = _alibi_slopes(N_HEADS)
    A = ATTN_WIDTH
    for l in range(DEPTH):
        x = x + 0.5 * _swiglu(_rmsnorm(x, ffn1_norm[l]), ffn1_w1[l], ffn1_w3[l], ffn1_w2[l])

        h = _rmsnorm(x, mix_norm[l])
        z = h @ w_in[l]
        zq, zk, zv = z[..., :A], z[..., A:2 * A], z[..., 2 * A:3 * A]
        za = z[..., 3 * A:3 * A + CONV_WIDTH]
        zg = z[..., 3 * A + CONV_WIDTH:]

        def heads(t):
            return t.reshape(B, S, N_HEADS, HEAD_DIM).transpose(0, 2, 1, 3)

        q = _rmsnorm(heads(zq), q_norm[l])
        k = _rmsnorm(heads(zk), k_norm[l])
        v = heads(zv)
        attn = _moba_attention(q, k, v, slopes)
        attn = attn.transpose(0, 2, 1, 3).reshape(B, S, A)

        conv = _conv_module(za, zg, conv_dw_w[l], conv_dw_b[l],
                            conv_ln_g[l], conv_ln_b[l])

        mixed = jnp.concatenate([attn, conv], axis=-1)
        x = x + mixed @ w_out[l]

        x = x + 0.5 * _swiglu(_rmsnorm(x, ffn2_norm[l]), ffn2_w1[l], ffn2_w3[l], ffn2_w2[l])
    return x
```

```python
import numpy as np
import ml_dtypes
from contextlib import ExitStack
import concourse.bass as bass
import concourse.mybir as mybir
from concourse.bass_utils import run_bass_kernel_spmd

F32 = mybir.dt.float32
BF16 = mybir.dt.bfloat16
AF = mybir.ActivationFunctionType
ALU = mybir.AluOpType
AX = mybir.AxisListType

D = 1024
DFF = 2816
NM = 22
T = 512
NEGB = -30000.0
EPS = 1e-6
NSLOT = 3
ENGS = ["pe", "act", "dve", "pool", "sp"]

C_G1, C_GM, C_G2, C_GQ, C_GK, C_DWB, C_LNG, C_LNB, C_DWW = 0, 8, 16, 24, 25, 26, 30, 34, 38
NSP = C_DWW + 124


class Res:
    __slots__ = ("name", "lw", "rd")

    def __init__(self, name):
        self.name = name
        self.lw = None
        self.rd = []


class Prog:
    def __init__(self, nc):
        self.nc = nc
        self.q = {e: [] for e in ENGS}
        self.tick = {e: 0 for e in ENGS}
        self.waited = {e: {} for e in ENGS}
        self.sems = {}
        self.dmacnt = {}

    def add_sem(self, name, handle):
        self.sems[name] = handle
        if name not in ENGS:
            self.dmacnt[name] = 0

    def _deps(self, eng, reads, writes):
        deps = {}

        def add(cv):
            if cv is None:
                return
            c, v = cv
            if deps.get(c, 0) < v:
                deps[c] = v
        for r in reads:
            add(r.lw)
        for w in writes:
            add(w.lw)
            for x in w.rd:
                add(x)
        out = []
        for c, v in deps.items():
            if c == eng and eng == "pe":
                continue
            if c in self.tick:
                assert v <= self.tick[c], ("wait on unsignalled tick", eng, c, v, self.tick[c])
            if self.waited[eng].get(c, 0) >= v:
                continue
            self.waited[eng][c] = v
            out.append((c, v))
        return out

    def _record(self, cv, reads, writes):
        for r in reads:
            r.rd.append(cv)
            if len(r.rd) > 64:
                m = {}
                for c, v in r.rd:
                    if m.get(c, 0) < v:
                        m[c] = v
                r.rd = list(m.items())
        for w in writes:
            w.lw = cv
            w.rd = []

    def emit(self, eng, fn, reads=(), writes=(), signal=True):
        waits = self._deps(eng, reads, writes)
        for c, v in waits:
            if c == eng:
                assert v <= self.tick[eng], (eng, v, self.tick[eng])
        if signal:
            self.tick[eng] += 1
            mytick = self.tick[eng]
        else:
            mytick = self.tick[eng] + 1
        sem = self.sems[eng]
        sems = self.sems

        def thunk(e):
            for c, v in waits:
                e.wait_ge(sems[c], v)
            ins = fn(e)
            if signal:
                ins.then_inc(sem, 1)
        self.q[eng].append(thunk)
        cv = (eng, mytick)
        self._record(cv, reads, writes)
        return cv

    def dma(self, eng, dsem, fn, reads=(), writes=()):
        waits = self._deps(eng, reads, writes)
        self.dmacnt[dsem] += 16
        val = self.dmacnt[dsem]
        sems = self.sems

        def thunk(e):
            for c, v in waits:
                e.wait_ge(sems[c], v)
            fn(e).then_inc(sems[dsem], 16)
        self.q[eng].append(thunk)
        cv = (dsem, val)
        self._record(cv, reads, writes)
        return cv

    def barrier(self):
        sems = self.sems
        for eng in ENGS:
            waits = []
            for c in ENGS:
                if c != eng and self.tick[c] > self.waited[eng].get(c, 0):
                    waits.append((c, self.tick[c]))
                    self.waited[eng][c] = self.tick[c]
            for c, v in self.dmacnt.items():
                if v > self.waited[eng].get(c, 0):
                    waits.append((c, v))
                    self.waited[eng][c] = v

            def thunk(e, waits=waits):
                for c, v in waits:
                    e.wait_ge(sems[c], v)
            self.q[eng].append(thunk)

    def wait_all(self, eng, ress):
        waits = self._deps(eng, (), ress)
        sems = self.sems

        def thunk(e):
            for c, v in waits:
                e.wait_ge(sems[c], v)
        self.q[eng].append(thunk)

    def run(self, block):
        q = self.q

        @block.tensor
        def _(e):
            for t in q["pe"]:
                t(e)

        @block.scalar
        def _(e):
            for t in q["act"]:
                t(e)

        @block.vector
        def _(e):
            for t in q["dve"]:
                t(e)

        @block.gpsimd
        def _(e):
            for t in q["pool"]:
                t(e)

        @block.sync
        def _(e):
            for t in q["sp"]:
                t(e)


def slab_descs():
    d = []
    for f in (0,):
        pass
    def ffn(f):
        for i in range(11):
            d.append(("up", f, i))
        for hf in range(2):
            for kg in range(3):
                d.append(("down", f, hf, kg))
    ffn(0)
    for nm in ("q", "k", "v", "za", "zg"):
        d.append(("win", nm))
    for c in range(4):
        d.append(("conv", c))
    for hf in range(2):
        d.append(("wout", hf))
    ffn(1)
    return d


class MK:
    def __init__(self, NSEQ, NT):
        self.NSEQ, self.NT = NSEQ, NT
        self.S = NT * T
        self.descs = slab_descs()
        self.NSLAB = len(self.descs)

    def build(self):
        nc = self.nc = bass.Bass("TRN2", target_bir_lowering=False)
        NSEQ, S = self.NSEQ, self.S
        dt = nc.dram_tensor
        self.x_d = dt("x", [NSEQ, S, D], F32, kind="ExternalInput").ap()
        self.w_d = {}
        for f in (0, 1):
            self.w_d[("w1", f)] = dt(f"f{f}w1", [D, DFF], F32, kind="ExternalInput").ap()
            self.w_d[("w3", f)] = dt(f"f{f}w3", [D, DFF], F32, kind="ExternalInput").ap()
            self.w_d[("w2", f)] = dt(f"f{f}w2", [DFF, D], F32, kind="ExternalInput").ap()
        self.win_d = dt("win", [D, 2560], F32, kind="ExternalInput").ap()
        self.wout_d = dt("wout", [D, D], F32, kind="ExternalInput").ap()
        self.sp_d = dt("sp", [128, NSP], F32, kind="ExternalInput").ap()
        self.idf_d = dt("identf", [128, 128], F32, kind="ExternalInput").ap()
        self.cbf_d = dt("cbf", [128, 512], BF16, kind="ExternalInput").ap()
        self.kaux_d = dt("kaux", [20, 4096], BF16, kind="ExternalInput").ap()
        self.qaux_d = dt("qaux", [8, 20, 4096], BF16, kind="ExternalInput").ap()
        self.out_d = dt("out", [NSEQ, S, D], F32, kind="ExternalOutput").ap()
        self.ws_d = dt("wstream", [self.NSLAB, 128, 4096], BF16).ap()

        with ExitStack() as es:
            def sb(name, shape, dtp):
                return es.enter_context(nc.sbuf_tensor(name, shape, dtp))

            def ps(name, shape, dtp):
                return es.enter_context(nc.psum_tensor(name, shape, dtp))
            self.xfm = sb("xfm", [128, 4096], F32)
            self.H = sb("H", [128, 2048], F32)
            self.A = sb("A", [128, NM * 512], BF16)
            self.Kc = [sb(f"Kc{h}", [84, 4096], BF16) for h in range(8)]
            self.Vc = sb("Vc", [128, 32 * 768], BF16)
            self.PT = sb("PT", [128, 6 * 512], BF16)
            self.Fall = sb("Fall", [128, 2048], F32)
            self.Fs = [self.Fall[:, i * 512:(i + 1) * 512] for i in range(4)]
            self.SQ = sb("SQ", [128, 2 * 512], BF16)
            self.ring = [sb(f"ring{i}", [128, 4096], BF16) for i in range(NSLOT)]
            self.spt = sb("spt", [128, NSP], F32)
            self.identf = sb("identf_sb", [128, 128], F32)
            self.cbf = sb("cbf_sb", [128, 512], BF16)
            self.cst = sb("cst", [128, 4], F32)
            self.kmT = sb("kmT", [64, 128], BF16)
            self.kmf = sb("kmf", [128, 2], F32)
            self.gsb = sb("gsb", [128, 128], F32)
            self.top8 = sb("top8", [128, 64], F32)
            self.msk = sb("msk", [128, 128], F32)
            self.mbw = sb("mbw", [128, 4 * 192], BF16)
            self.halo = sb("halo", [128, 4 * 30], BF16)
            self.pb = [ps(f"pb{i}", [128, 512], F32) for i in range(8)]

            P = self.P = Prog(nc)
            for e in ENGS:
                P.add_sem(e, es.enter_context(nc.semaphore("s_" + e)))
            dsems = ["cst", "ka", "qa", "st0", "st1"] + [f"xl{i}" for i in range(4)] + [f"xs{i}" for i in range(4)] + \
                [f"wl{i}" for i in range(NSLOT)] + [f"ws{i}" for i in range(NSLOT)]
            for dname in dsems:
                P.add_sem(dname, es.enter_context(nc.semaphore("d_" + dname)))
            block = es.enter_context(nc.Block())

            self.Rx = [Res(f"x{k}") for k in range(8)]
            self.RH = [Res(f"H{k}") for k in range(8)]
            self.RA = [Res(f"A{k}") for k in range(NM)]
            self.RK = [Res(f"K{h}") for h in range(8)]
            self.RV = Res("V")
            self.RPT = [Res(f"PT{i}") for i in range(6)]
            self.RF = [Res(f"F{i}") for i in range(4)]
            self.RSQ = [Res(f"SQ{i}") for i in range(2)]
            self.Rring = [Res(f"ring{i}") for i in range(NSLOT)]
            self.Rc = Res("consts")
            self.Rcst = Res("cst")
            self.Rcst2 = Res("cst2")
            self.Rcst3 = Res("cst3")
            self.Rkm = Res("kmT")
            self.Rkmf = Res("kmf")
            self.Rgsb = Res("gsb")
            self.Rtop = Res("top8")
            self.Rmsk = Res("msk")
            self.Rmbw = [Res(f"mbw{i}") for i in range(4)]
            self.Rpb = [Res(f"pb{i}") for i in range(8)]
            self.Rws = Res("wstream")
            self.Rslab = [Res(f"slab{i}") for i in range(self.NSLAB)]
            self.Rhalo = Res("halo")
            self.Rout = Res("out")
            self.rr = 0
            self.ptr = 0

            self.Hb = self.H[:].bitcast(BF16)
            self.ident_bf = self.cbf[:, 0:128]
            self.ones_bf = self.cbf[:, 128:256]
            self.bones_bf = self.cbf[:, 256:384]
            self.tri_bf = self.cbf[:, 384:512]

            self.phase0()
            self.init_main()
            self.wseq = [i for _ in range(NSEQ * self.NT) for i in range(self.NSLAB)]
            self.wpos = 0
            for s in range(NSEQ):
                self.seq_init(s)
                for t in range(self.NT):
                    self.tile(s, t)
            P.barrier()
            P.run(block)
        return nc

    def xk(self, k):
        return self.xfm[:, k * 512:(k + 1) * 512]

    def hk(self, k):
        return self.Hb[:, k * 512:(k + 1) * 512]

    def ak(self, m):
        return self.A[:, m * 512:(m + 1) * 512]

    def qa(self, h, rows=84):
        return self.A[0:rows, h * 512:(h + 1) * 512]

    def ut(self, c):
        return self.A[:, 4096 + c * 542: 4096 + (c + 1) * 542]

    R_UT = property(lambda self: self.RA[8:13])

    def ybf(self, c):
        return self.ak(13 + c)

    def ysq(self, c):
        return self.ak(17 + c)

    def bank(self):
        i = self.rr
        self.rr = (self.rr + 1) % 6
        return i

    def spc(self, c, lo=0, hi=128):
        return self.spt[lo:hi, c:c + 1]

    def eng_alt(self):
        self._alt = getattr(self, "_alt", 0) ^ 1
        return "act" if self._alt else "dve"

    def copy_any(self, eng, out, in_, reads, writes):
        if eng == "act":
            self.P.emit("act", lambda e: e.activation(out=out, in_=in_, func=AF.Copy), reads, writes)
        else:
            self.P.emit(eng, lambda e: e.tensor_copy(out=out, in_=in_), reads, writes)

    def scale_cast(self, eng, out, in_, col, reads, writes):
        if eng == "act":
            self.P.emit("act", lambda e: e.activation(out=out, in_=in_, func=AF.Copy, scale=col), reads, writes)
        else:
            self.P.emit("dve", lambda e: e.tensor_scalar(out=out, in0=in_, scalar1=col, scalar2=None, op0=ALU.mult),
                        reads, writes)

    def phase0(self):
        P = self.P
        P.dma("sp", "cst", lambda e: e.dma_start(out=self.spt[:], in_=self.sp_d[:, :]), writes=[self.Rc])
        P.dma("sp", "cst", lambda e: e.dma_start(out=self.identf[:], in_=self.idf_d[:, :]), writes=[self.Rc])
        P.dma("sp", "cst", lambda e: e.dma_start(out=self.cbf[:], in_=self.cbf_d[:, :]), writes=[self.Rc])
        vf = self.Vc[:].bitcast(F32)
        stg = [vf[:, 4096:8192], vf[:, 8192:12288]]
        Rst = self.Rst = [Res("stg0"), Res("stg1")]
        par, k = [], 0
        for desc in self.descs:
            par.append(k)
            if desc[0] != "conv":
                k ^= 1

        def loads(idx):
            desc = self.descs[idx]
            kk_ = par[idx]
            st, Rs, ds = stg[kk_], Rst[kk_], f"st{kk_}"
            kind = desc[0]
            if kind == "up":
                f, i = desc[1], desc[2]
                stv = st.rearrange("p (w kc n) -> p w kc n", w=2, kc=8, n=256)
                for w, nm in enumerate(("w1", "w3")):
                    src = self.w_d[(nm, f)].rearrange("(kc p) n -> p kc n", p=128)[:, :, 256 * i:256 * i + 256]
                    P.dma("sp", ds, lambda e, o=stv[:, w], s_=src: e.dma_start(out=o, in_=s_), writes=[Rs])
            elif kind == "down":
                f, hf, kg = desc[1], desc[2], desc[3]
                nk = min(8, NM - 8 * kg)
                src = self.w_d[("w2", f)].rearrange("(kc p) n -> p kc n", p=128)[:, 8 * kg:8 * kg + nk, 512 * hf:512 * hf + 512]
                stv = st[:, 0:nk * 512].rearrange("p (kc c) -> p kc c", kc=nk)
                P.dma("sp", ds, lambda e, o=stv, s_=src: e.dma_start(out=o, in_=s_), writes=[Rs])
            elif kind == "win":
                c0 = {"q": 0, "k": 512, "v": 1024, "za": 1536, "zg": 2048}[desc[1]]
                src = self.win_d.rearrange("(kc p) n -> p kc n", p=128)[:, :, c0:c0 + 512]
                stv = st.rearrange("p (kc n) -> p kc n", kc=8)
                P.dma("sp", ds, lambda e, o=stv, s_=src: e.dma_start(out=o, in_=s_), writes=[Rs])
            elif kind == "wout":
                hf = desc[1]
                src = self.wout_d.rearrange("(kc p) n -> p kc n", p=128)[:, :, hf * 512:hf * 512 + 512]
                stv = st.rearrange("p (kc n) -> p kc n", kc=8)
                P.dma("sp", ds, lambda e, o=stv, s_=src: e.dma_start(out=o, in_=s_), writes=[Rs])

        def casts(idx):
            desc = self.descs[idx]
            slot = idx % NSLOT
            sl = self.ring[slot][:]
            Rsl = self.Rring[slot]
            kk_ = par[idx]
            st, Rs = stg[kk_], Rst[kk_]
            kind = desc[0]
            if kind == "up":
                f, i = desc[1], desc[2]
                gc = C_G1 if f == 0 else C_G2
                stv = st.rearrange("p (w kc n) -> p w kc n", w=2, kc=8, n=256)
                slv = sl.rearrange("p (j w kc c) -> p j w kc c", j=2, w=2, kc=8, c=128)
                for w in range(2):
                    for kc in range(8):
                        self.scale_cast(self.eng_alt(), slv[:, :, w, kc, :],
                                        stv[:, w, kc, :].rearrange("p (j c) -> p j c", j=2),
                                        self.spc(gc + kc), [Rs, self.Rc], [Rsl])
            elif kind == "down":
                nk = min(8, NM - 8 * desc[3])
                self.copy_any(self.eng_alt(), sl[:, 0:nk * 512], st[:, 0:nk * 512], [Rs], [Rsl])
            elif kind == "win":
                nm = desc[1]
                stv = st.rearrange("p (kc n) -> p kc n", kc=8)
                for kc in range(8):
                    if nm == "v":
                        o = sl[:, kc * 512:(kc + 1) * 512]
                        i_ = stv[:, kc, :]
                    else:
                        o = sl.rearrange("p (m kc c) -> p m kc c", m=4, kc=8)[:, :, kc, :]
                        i_ = stv[:, kc, :].rearrange("p (m c) -> p m c", m=4)
                    self.scale_cast(self.eng_alt(), o, i_, self.spc(C_GM + kc), [Rs, self.Rc], [Rsl])
            elif kind == "conv":
                c = desc[1]
                for kk in range(31):
                    self.scale_cast(self.eng_alt(), sl[:, kk * 128:(kk + 1) * 128], self.identf[:],
                                    self.spc(C_DWW + c * 31 + kk), [self.Rc], [Rsl])
            elif kind == "wout":
                stv = st.rearrange("p (kc n) -> p kc n", kc=8)
                for kc in range(8):
                    o = sl.rearrange("p (m kc c) -> p m kc c", m=4, kc=8)[:, :, kc, :]
                    i_ = stv[:, kc, :].rearrange("p (m c) -> p m c", m=4)
                    self.copy_any(self.eng_alt(), o, i_, [Rs], [Rsl])
            P.dma("pool", f"ws{slot}", lambda e, o=self.ws_d[idx, :, :], s_=sl: e.dma_start(out=o, in_=s_),
                  reads=[Rsl], writes=[self.Rslab[idx]])

        self._p0_loads, self._p0_casts = loads, casts
        self.p0_loaded = 0
        self.p0_cast = 0

    def p0_advance(self, upto):
        n = self.NSLAB
        upto = min(upto, n)
        while self.p0_cast < upto:
            while self.p0_loaded < min(self.p0_cast + 2, n):
                self._p0_loads(self.p0_loaded)
                self.p0_loaded += 1
            self._p0_casts(self.p0_cast)
            self.p0_cast += 1

    def init_main(self):
        P = self.P
        vv = self.Vc[:].rearrange("p (c a s d) -> p c a s d", c=32, a=4, s=3, d=64)
        for c in range(10):
            P.emit("pool", lambda e, o=vv[:, c, :, 1, :]: e.memset(o, 1.0), writes=[self.RV])
        for h in range(8):
            P.dma("sp", "ka", lambda e, h=h: e.dma_start(out=self.Kc[h][64:84, :], in_=self.kaux_d[:, :]),
                  writes=[self.RK[h]])
        for h in range(8):
            self.RK[h].lw = ("ka", P.dmacnt["ka"])
        P.emit("pool", lambda e: e.memset(self.mbw[:], 0.0), writes=self.Rmbw)
        P.emit("dve", lambda e: e.memset(self.cst[:, 0:1], EPS), writes=[self.Rcst])
        P.emit("dve", lambda e: e.memset(self.cst[:, 2:3], 1.0), writes=[self.Rcst2])
        P.emit("dve", lambda e: e.tensor_scalar(out=self.cst[:, 1:2], in0=self.spc(C_GQ), scalar1=0.125,
                                                scalar2=None, op0=ALU.mult), reads=[self.Rc], writes=[self.Rcst])

    def seq_init(self, s):
        P = self.P
        P.emit("pool", lambda e: e.memset(self.halo[:], 0.0), writes=[self.Rhalo])
        P.emit("pool", lambda e: e.memset(self.gsb[:], NEGB), writes=[self.Rgsb])
        P.emit("pool", lambda e: e.memset(self.kmT[:], 0.0), writes=[self.Rkm])

    def _wload(self, j):
        if j < self.NSLAB:
            return
        slot = j % NSLOT
        idx = self.wseq[j]
        self.P.dma("sp", f"wl{slot}", lambda e: e.dma_start(out=self.ring[slot][:], in_=self.ws_d[idx, :, :]),
                   reads=[self.Rslab[idx]], writes=[self.Rring[slot]])

    def wnext(self, expect):
        j = self.wpos
        assert self.descs[self.wseq[j]] == expect, (self.descs[self.wseq[j]], expect)
        if j < self.NSLAB:
            self.p0_advance(j + NSLOT)
        slot = j % NSLOT
        return self.ring[slot], self.Rring[slot]

    def wdone(self):
        j = self.wpos
        self.wpos += 1
        if j + NSLOT < len(self.wseq):
            self._wload(j + NSLOT)

    def mm(self, out, lhsT, rhs, start, stop, reads, writes, signal=None):
        if signal is None:
            signal = stop
        self.P.emit("pe", lambda e: e.matmul(out, lhsT=lhsT, rhs=rhs, start=start, stop=stop),
                    reads, writes, signal)

    def stg_in(self, i):
        if i < 2:
            return self.H[:, i * 1024:(i + 1) * 1024], self.RH[4 * i:4 * i + 4]
        j = i - 2
        return self.Fall[:, j * 1024:(j + 1) * 1024], self.RF[2 * j:2 * j + 2]

    def stg_out(self, i):
        return self.A[:].bitcast(F32)[:, i * 1024:(i + 1) * 1024], self.RA[4 * i:4 * i + 4]

    def prefetch_x(self, s, t):
        for tc in range(4):
            st, Rst = self.stg_in(tc)
            r0 = t * T + tc * 128
            self.P.dma("pool", f"xl{tc}", lambda e, o=st, s_=self.x_d[s, r0:r0 + 128, :]: e.dma_start(out=o, in_=s_),
                       writes=Rst)

    def load_x(self, s, t):
        P = self.P
        self.norm_begin()
        self.preload_ln()
        pend = None
        for kc in range(8):
            b = self.bank()
            for tc in range(4):
                st, Rst = self.stg_in(tc)
                P.emit("pe", lambda e, o=self.pb[b][:, tc * 128:(tc + 1) * 128], i_=st[:, kc * 128:(kc + 1) * 128]:
                       e.transpose(out=o, in_=i_, identity=self.identf[:]),
                       reads=Rst + [self.Rc], writes=[self.Rpb[b]], signal=(tc == 3))
            self.copy_any("dve", self.xk(kc), self.pb[b][:], [self.Rpb[b]], [self.Rx[kc]])
            if pend is not None:
                self.norm_chunk(pend)
            pend = kc
        self.norm_chunk(pend)

    def store_x(self, s, t):
        P = self.P
        for tc in range(4):
            st, Rst = self.stg_out(tc)
            r0 = t * T + tc * 128
            for g in range(2):
                b = self.bank()
                for kl in range(4):
                    kc = g * 4 + kl
                    P.emit("pe", lambda e, o=self.pb[b][:, kl * 128:(kl + 1) * 128],
                           i_=self.xk(kc)[:, tc * 128:(tc + 1) * 128]:
                           e.transpose(out=o, in_=i_, identity=self.identf[:]),
                           reads=[self.Rx[kc], self.Rc], writes=[self.Rpb[b]], signal=(kl == 3))
                self.copy_any(self.eng_alt(), st[:, g * 512:(g + 1) * 512], self.pb[b][:], [self.Rpb[b]], Rst)
            P.dma("pool", f"xs{tc}", lambda e, o=self.out_d[s, r0:r0 + 128, :], s_=st: e.dma_start(out=o, in_=s_),
                  reads=Rst, writes=[self.Rout])

    def rstd_from(self, bnk, inv_n, Fi):
        P = self.P
        F, RF = self.Fs[Fi], self.RF[Fi]
        P.emit("act", lambda e: e.activation(out=F[:], in_=self.pb[bnk][:], func=AF.Ln, scale=inv_n,
                                             bias=self.cst[:, 0:1]),
               reads=[self.Rpb[bnk], self.Rcst], writes=[RF])
        P.emit("act", lambda e: e.activation(out=F[:], in_=F[:], func=AF.Exp, scale=-0.5), reads=[RF], writes=[RF])

    def preload_ln(self):
        self.P.emit("act", lambda e: e.activation(out=self.cst[:, 3:4], in_=self.cst[:, 2:3], func=AF.Ln),
                    reads=[self.Rcst2], writes=[self.Rcst3])

    def norm_begin(self):
        self.ncount = 0

    def norm_chunk(self, kc, from_bank=None):
        P = self.P
        i = self.ncount % 2
        if from_bank is None:
            src, Rsrc = self.xk(kc), self.Rx[kc]
        else:
            src, Rsrc = self.pb[from_bank][:], self.Rpb[from_bank]
        P.emit("act", lambda e, o=self.SQ[:, i * 512:(i + 1) * 512], i_=src: e.activation(out=o, in_=i_, func=AF.Square),
               reads=[Rsrc], writes=[self.RSQ[i]])
        self.mm(self.pb[6][:], self.ones_bf, self.SQ[:, i * 512:(i + 1) * 512], self.ncount == 0, self.ncount == 7,
                [self.RSQ[i], self.Rc], [self.Rpb[6]], signal=True)
        self.ncount += 1

    def norm_finish(self):
        P = self.P
        assert self.ncount == 8
        self.rstd_from(6, 1.0 / D, 3)
        for kc in range(8):
            eng = "pool" if kc in (2, 4, 6) else "dve"
            P.emit(eng, lambda e, o=self.hk(kc), i_=self.xk(kc): e.tensor_tensor(out=o, in0=i_, in1=self.Fs[3][:], op=ALU.mult),
                   reads=[self.Rx[kc], self.RF[3]], writes=[self.RH[kc]])

    def ffn(self, f, after_up=None):
        P = self.P
        self.norm_finish()
        for i in range(11):
            sl, Rsl = self.wnext(("up", f, i))
            slv = sl[:].rearrange("p (j w kc c) -> p j w kc c", j=2, w=2, kc=8, c=128)
            pre = None
            if i == 0:
                pre = [(self.bank(), self.bank()) for _ in range(2)]
                for kc in range(8):
                    for j in range(2):
                        for w in range(2):
                            bk = pre[j][w]
                            self.mm(self.pb[bk][:], slv[:, j, w, kc, :], self.hk(kc), kc == 0, kc == 7,
                                    [Rsl, self.RH[kc]], [self.Rpb[bk]])
            for j in range(2):
                m = 2 * i + j
                if pre is not None:
                    ba, bb = pre[j]
                else:
                    ba, bb = self.bank(), self.bank()
                    for w, bk in ((0, ba), (1, bb)):
                        for kc in range(8):
                            self.mm(self.pb[bk][:], slv[:, j, w, kc, :], self.hk(kc), kc == 0, kc == 7,
                                    [Rsl, self.RH[kc]], [self.Rpb[bk]])
                Fi = m % 2
                P.emit("act", lambda e, o=self.Fs[Fi][:], i_=self.pb[ba][:]: e.activation(out=o, in_=i_, func=AF.Silu),
                       reads=[self.Rpb[ba]], writes=[self.RF[Fi]])
                P.emit("dve", lambda e, o=self.ak(m), i0=self.pb[bb][:], i1=self.Fs[Fi][:]:
                       e.tensor_tensor(out=o, in0=i0, in1=i1, op=ALU.mult),
                       reads=[self.Rpb[bb], self.RF[Fi]], writes=[self.RA[m]])
            self.wdone()
        if after_up is not None:
            after_up()
        if f == 0:
            self.norm_begin()
            self.preload_ln()
        pend = []
        for hf in range(2):
            banks = [self.bank() for _ in range(4)]
            for kg in range(3):
                sl, Rsl = self.wnext(("down", f, hf, kg))
                nk = min(8, NM - 8 * kg)
                for ml in range(4):
                    bk = banks[ml]
                    for kl in range(nk):
                        kc = 8 * kg + kl
                        self.mm(self.pb[bk][:], sl[:, kl * 512 + ml * 128:kl * 512 + (ml + 1) * 128], self.ak(kc),
                                kc == 0, kc == NM - 1, [Rsl, self.RA[kc]], [self.Rpb[bk]],
                                signal=(kc == NM - 1) or (ml == 3 and kl == nk - 1))
                self.wdone()
            for ml in range(4):
                mo = hf * 4 + ml
                P.emit("dve", lambda e, o=self.xk(mo), i0=self.pb[banks[ml]][:]:
                       e.scalar_tensor_tensor(out=o, in0=i0, scalar=0.5, in1=o, op0=ALU.mult, op1=ALU.add),
                       reads=[self.Rpb[banks[ml]], self.Rx[mo]], writes=[self.Rx[mo]])
            if f == 0:
                for mo in pend:
                    self.norm_chunk(mo)
                pend = [hf * 4 + ml for ml in range(4)]
        if f == 0:
            for mo in pend:
                self.norm_chunk(mo)

    def qk_chunks(self, s, t, which):
        P = self.P
        sl, Rsl = self.wnext(("win", which))
        slv = sl[:].rearrange("p (m kc c) -> p m kc c", m=4, kc=8)
        col0 = t * T
        pend = None

        def finish(c, bz):
            i = c % 2
            bs = self.bank()
            self.mm(self.pb[bs][:], self.bones_bf, self.SQ[:, i * 512:(i + 1) * 512], True, True,
                    [self.RSQ[i], self.Rc], [self.Rpb[bs]])
            Fi = c % 2
            self.rstd_from(bs, 1.0 / 64, Fi)
            for hh in range(2):
                h = 2 * c + hh
                lo, hi = hh * 64, hh * 64 + 64
                if which == "q":
                    o, Ro = self.qa(h, 64), [self.RA[h]]
                    colap, Rcol = self.cst[lo:hi, 1:2], self.Rcst
                else:
                    o, Ro = self.Kc[h][0:64, col0:col0 + T], [self.RK[h]]
                    colap, Rcol = self.spc(C_GK, lo, hi), self.Rc
                P.emit("dve", lambda e, o=o, i0=self.pb[bz][lo:hi, :], i1=self.Fs[Fi][lo:hi, :], colap=colap:
                       e.scalar_tensor_tensor(out=o, in0=i0, scalar=colap, in1=i1, op0=ALU.mult, op1=ALU.mult),
                       reads=[self.Rpb[bz], self.RF[Fi], Rcol], writes=Ro)
        pre = {}
        if which == "q":
            pre = {0: self.bank(), 1: self.bank()}
            for kc in range(8):
                for c in (0, 1):
                    self.mm(self.pb[pre[c]][:], slv[:, c, kc, :], self.hk(kc), kc == 0, kc == 7,
                            [Rsl, self.RH[kc]], [self.Rpb[pre[c]]])
        for c in range(4):
            if c in pre:
                bz = pre[c]
            else:
                bz = self.bank()
                for kc in range(8):
                    self.mm(self.pb[bz][:], slv[:, c, kc, :], self.hk(kc), kc == 0, kc == 7,
                            [Rsl, self.RH[kc]], [self.Rpb[bz]])
            i = c % 2
            P.emit("act", lambda e, o=self.SQ[:, i * 512:(i + 1) * 512], i_=self.pb[bz][:]: e.activation(out=o, in_=i_, func=AF.Square),
                   reads=[self.Rpb[bz]], writes=[self.RSQ[i]])
            if pend is not None:
                finish(*pend)
            pend = (c, bz)
        self.wdone()
        finish(*pend)

    def v_chunks(self, s, t):
        sl, Rsl = self.wnext(("win", "v"))
        for tc in range(4):
            b = self.bank()
            for kc in range(8):
                self.mm(self.pb[b][:], self.hk(kc)[:, tc * 128:(tc + 1) * 128], sl[:, kc * 512:(kc + 1) * 512],
                        kc == 0, kc == 7, [Rsl, self.RH[kc]], [self.Rpb[b]])
            ch = t * 4 + tc
            o = self.Vc[:, ch * 768:(ch + 1) * 768].rearrange("p (a s d) -> p a s d", a=4, s=3, d=64)[:, :, 0:3:2, :]
            i_ = self.pb[b][:].rearrange("p (a s d) -> p a s d", a=4, s=2, d=64)
            self.copy_any(self.eng_alt(), o, i_, [self.Rpb[b]], [self.RV])
        self.wdone()

    def conv_in(self, s, t):
        P = self.P
        for c in range(4):
            P.emit("pool", lambda e, o=self.ut(c)[:, 0:30], i_=self.halo[:, c * 30:(c + 1) * 30]: e.tensor_copy(out=o, in_=i_),
                   reads=[self.Rhalo], writes=self.R_UT)
        sla, Rsla = self.wnext(("win", "za"))
        slav = sla[:].rearrange("p (m kc c) -> p m kc c", m=4, kc=8)
        banks = []
        for c in range(4):
            b = self.bank()
            for kc in range(8):
                self.mm(self.pb[b][:], slav[:, c, kc, :], self.hk(kc), kc == 0, kc == 7,
                        [Rsla, self.RH[kc]], [self.Rpb[b]])
            banks.append(b)
        self.wdone()
        slg, Rslg = self.wnext(("win", "zg"))
        slgv = slg[:].rearrange("p (m kc c) -> p m kc c", m=4, kc=8)
        for c in range(4):
            b = 6 + (c % 2)
            for kc in range(8):
                self.mm(self.pb[b][:], slgv[:, c, kc, :], self.hk(kc), kc == 0, kc == 7,
                        [Rslg, self.RH[kc]], [self.Rpb[b]])
            Fi = c % 2
            P.emit("act", lambda e, o=self.Fs[Fi][:], i_=self.pb[b][:]: e.activation(out=o, in_=i_, func=AF.Sigmoid),
                   reads=[self.Rpb[b]], writes=[self.RF[Fi]])
            P.emit("dve", lambda e, o=self.ut(c)[:, 30:542], i0=self.pb[banks[c]][:], i1=self.Fs[Fi][:]:
                   e.tensor_tensor(out=o, in0=i0, in1=i1, op=ALU.mult),
                   reads=[self.Rpb[banks[c]], self.RF[Fi]], writes=self.R_UT)
        self.wdone()

    def kmean(self, s, t):
        P = self.P
        col0 = t * T
        for h in range(8):
            kin = self.Kc[h][0:64, col0:col0 + T].rearrange("p (b k) -> p b k", b=2)
            P.emit("dve", lambda e, i_=kin: e.tensor_reduce(out=self.kmf[0:64, :], in_=i_, axis=AX.X, op=ALU.add),
                   reads=[self.RK[h]], writes=[self.Rkmf])
            P.emit("dve", lambda e, o=self.kmT[0:64, h * 16 + 2 * t:h * 16 + 2 * t + 2]:
                   e.tensor_scalar(out=o, in0=self.kmf[0:64, :], scalar1=1.0 / 256, scalar2=None, op0=ALU.mult),
                   reads=[self.Rkmf], writes=[self.Rkm])

    def gate_scores(self, s, t):
        P = self.P
        if t < 2:
            return
        for qc in range(4):
            b = 2 * t + qc // 2
            g = self.bank()
            for h in range(8):
                self.mm(self.pb[g][:, h * 16:(h + 1) * 16], self.qa(h, 64)[:, qc * 128:(qc + 1) * 128],
                        self.kmT[0:64, h * 16:(h + 1) * 16], True, True,
                        [self.RA[h], self.Rkm], [self.Rpb[g]], signal=(h == 7))
            gv = self.gsb[:].rearrange("p (h n) -> p h n", h=8)
            P.emit("dve", lambda e, o=gv[:, :, 0:b], i_=self.pb[g][:, 0:128].rearrange("p (h n) -> p h n", h=8)[:, :, 0:b]:
                   e.tensor_copy(out=o, in_=i_), reads=[self.Rpb[g]], writes=[self.Rgsb])
            for h in range(8):
                P.emit("dve", lambda e, o=self.top8[:, h * 8:(h + 1) * 8], i_=self.gsb[:, h * 16:(h + 1) * 16]:
                       e.max(out=o, in_=i_), reads=[self.Rgsb], writes=[self.Rtop])
            thr = self.top8[:].rearrange("p (h k) -> p h k", h=8)[:, :, 2:3].broadcast_to([128, 8, 16])
            P.emit("dve", lambda e, thr=thr: e.tensor_tensor(out=self.msk[:].rearrange("p (h n) -> p h n", h=8), in0=gv, in1=thr,
                                                            op=ALU.is_ge),
                   reads=[self.Rgsb, self.Rtop], writes=[self.Rmsk])
            mo = self.mbw[:, qc * 192 + 64:qc * 192 + 192]
            P.emit("dve", lambda e, mo=mo: e.tensor_scalar(out=mo, in0=self.msk[:], scalar1=1.0, scalar2=-NEGB,
                                                          op0=ALU.subtract, op1=ALU.mult),
                   reads=[self.Rmsk], writes=[self.Rmbw[qc]])
            P.emit("dve", lambda e, o=mo.rearrange("p (h n) -> p h n", h=8)[:, :, b:b + 1]: e.memset(o, 0.0),
                   writes=[self.Rmbw[qc]])

    def gate_rows(self, s, t):
        P = self.P
        if t < 2:
            return
        for h in range(8):
            g = self.bank()
            gb = self.pb[g][:].bitcast(BF16)
            for qc in range(4):
                P.emit("pe", lambda e, o=gb[0:80, qc * 128:(qc + 1) * 128],
                       i_=self.mbw[:, qc * 192 + 16 * h:qc * 192 + 16 * h + 80]:
                       e.transpose(out=o, in_=i_, identity=self.ident_bf),
                       reads=[self.Rmbw[qc], self.Rc], writes=[self.Rpb[g]], signal=(qc == 3))
            self.copy_any("dve", self.qa(h)[64:80, :], gb[64:80, 0:512], [self.Rpb[g]], [self.RA[h]])

    def attention(self, s, t, between=None):
        P = self.P
        b0, b1 = 2 * t, 2 * t + 1
        for h in range(8):
            acc = 6 + (h % 2)
            pair, odd = h // 2, h % 2
            vlo = pair * 192 + (64 if odd else 0)
            qh = self.qa(h)
            chunks = []
            for n in range(b0):
                for kc in range(2):
                    chunks.append((n * 256 + kc * 128, 0, 512, False))
            chunks.append((b0 * 256, 0, 512, True))
            chunks.append((b0 * 256 + 128, 128, 512, True))
            chunks.append((b1 * 256, 256, 512, True))
            chunks.append((b1 * 256 + 128, 384, 512, True))
            staged = []

            def qk(ci):
                k0, ql, qhi, tri = chunks[ci]
                n_ = qhi - ql
                sb_ = self.bank()
                self.mm(self.pb[sb_][:, 0:n_], self.Kc[h][0:84, k0:k0 + 128], qh[:, ql:qhi],
                        True, not tri, [self.RK[h], self.RA[h]], [self.Rpb[sb_]])
                if tri:
                    self.mm(self.pb[sb_][:, 0:128], self.ident_bf, self.tri_bf, False, True,
                            [self.Rc], [self.Rpb[sb_]])
                pi = self.ptr
                self.ptr = (self.ptr + 1) % 6
                P.emit("act", lambda e, o=self.PT[:, pi * 512:pi * 512 + n_], i_=self.pb[sb_][:, 0:n_]:
                       e.activation(out=o, in_=i_, func=AF.Exp),
                       reads=[self.Rpb[sb_]], writes=[self.RPT[pi]])
                staged.append((ci, pi))

            def pv(first, last):
                ci, pi = staged.pop(0)
                k0, ql, qhi, tri = chunks[ci]
                n_ = qhi - ql
                ch = k0 // 128
                self.mm(self.pb[acc][:, ql:qhi], self.Vc[:, ch * 768 + vlo:ch * 768 + vlo + 128],
                        self.PT[:, pi * 512:pi * 512 + n_], first, last,
                        [self.RV, self.RPT[pi]], [self.Rpb[acc]], signal=(last or ci + LOOK >= nch))
            nch = len(chunks)
            LOOK = 4
            for ci in range(min(LOOK, nch)):
                qk(ci)
            for ci in range(nch):
                pv(ci == 0, ci == nch - 1)
                if ci + LOOK < nch:
                    qk(ci + LOOK)
            alo, dlo = (64, 0) if odd else (0, 64)
            rec = self.Fs[2][alo:alo + 64, :]
            if h == 7:
                P.emit("act", lambda e, rec=rec, i_=self.pb[acc][dlo:dlo + 64, :]: e.activation(out=rec, in_=i_, func=AF.Ln),
                       reads=[self.Rpb[acc]], writes=[self.RF[2]])
                P.emit("act", lambda e, rec=rec: e.activation(out=rec, in_=rec, func=AF.Exp, scale=-1.0),
                       reads=[self.RF[2]], writes=[self.RF[2]])
            else:
                P.emit("dve", lambda e, rec=rec, i_=self.pb[acc][dlo:dlo + 64, :]: e.reciprocal(out=rec, in_=i_),
                       reads=[self.Rpb[acc]], writes=[self.RF[2]])
            o = self.hk(pair)[alo:alo + 64, :]
            P.emit("dve", lambda e, o=o, i0=self.pb[acc][alo:alo + 64, :], rec=rec:
                   e.tensor_tensor(out=o, in0=i0, in1=rec, op=ALU.mult),
                   reads=[self.Rpb[acc], self.RF[2]], writes=[self.RH[pair]])
            if between and h in between:
                between[h]()

    def conv_mm(self, c):
        P = self.P
        sl, Rsl = self.wnext(("conv", c))
        b = self.bank()
        for kk in range(31):
            self.mm(self.pb[b][:], sl[:, kk * 128:(kk + 1) * 128], self.ut(c)[:, kk:kk + 512], kk == 0, kk == 30,
                    [Rsl] + self.R_UT, [self.Rpb[b]])
        self.wdone()
        P.emit("dve", lambda e, o=self.ybf(c), i_=self.pb[b][:], bc=self.spc(C_DWB + c):
               e.tensor_scalar(out=o, in0=i_, scalar1=bc, scalar2=None, op0=ALU.add),
               reads=[self.Rpb[b], self.Rc], writes=[self.RA[13 + c]])
        P.emit("dve", lambda e, o=self.ysq(c), i_=self.pb[b][:], bc=self.spc(C_DWB + c), y=self.ybf(c):
               e.scalar_tensor_tensor(out=o, in0=i_, scalar=bc, in1=y, op0=ALU.add, op1=ALU.mult),
               reads=[self.Rpb[b], self.Rc, self.RA[13 + c]], writes=[self.RA[17 + c]])
        P.emit("pool", lambda e, o=self.halo[:, c * 30:(c + 1) * 30], i_=self.ut(c)[:, 512:542]: e.tensor_copy(out=o, in_=i_),
               reads=self.R_UT, writes=[self.Rhalo])

    def conv_stats(self):
        P = self.P
        b1, b2 = self.bank(), self.bank()
        for c in range(4):
            self.mm(self.pb[b1][:], self.ones_bf, self.ybf(c), c == 0, c == 3, [self.Rc, self.RA[13 + c]], [self.Rpb[b1]])
        for c in range(4):
            self.mm(self.pb[b2][:], self.ones_bf, self.ysq(c), c == 0, c == 3, [self.Rc, self.RA[17 + c]], [self.Rpb[b2]])
        M, T1 = self.Fs[0], self.Fs[1]
        P.emit("dve", lambda e: e.tensor_scalar(out=M[:], in0=self.pb[b1][:], scalar1=1.0 / 512, scalar2=None, op0=ALU.mult),
               reads=[self.Rpb[b1]], writes=[self.RF[0]])
        P.emit("dve", lambda e: e.tensor_tensor(out=T1[:], in0=M[:], in1=M[:], op=ALU.mult),
               reads=[self.RF[0]], writes=[self.RF[1]])
        P.emit("dve", lambda e: e.scalar_tensor_tensor(out=T1[:], in0=self.pb[b2][:], scalar=1.0 / 512, in1=T1[:],
                                                       op0=ALU.mult, op1=ALU.subtract),
               reads=[self.Rpb[b2], self.RF[1]], writes=[self.RF[1]])
        P.emit("dve", lambda e: e.tensor_scalar(out=T1[:], in0=T1[:], scalar1=0.0, scalar2=None, op0=ALU.max),
               reads=[self.RF[1]], writes=[self.RF[1]])

    def conv_rstd(self):
        P = self.P
        T1, RS = self.Fs[1], self.Fs[3]
        P.emit("act", lambda e: e.activation(out=RS[:], in_=T1[:], func=AF.Ln, bias=self.cst[:, 0:1]),
               reads=[self.RF[1], self.Rcst], writes=[self.RF[3]])
        P.emit("act", lambda e: e.activation(out=RS[:], in_=RS[:], func=AF.Exp, scale=-0.5),
               reads=[self.RF[3]], writes=[self.RF[3]])

    def conv_apply(self):
        P = self.P
        M, RS = self.Fs[0], self.Fs[3]
        for c in range(4):
            P.emit("dve", lambda e, y=self.ysq(c), i0=self.ybf(c): e.tensor_tensor(out=y, in0=i0, in1=M[:], op=ALU.subtract),
                   reads=[self.RA[13 + c], self.RF[0]], writes=[self.RA[17 + c]])
            P.emit("dve", lambda e, y=self.ysq(c): e.tensor_tensor(out=y, in0=y, in1=RS[:], op=ALU.mult),
                   reads=[self.RA[17 + c], self.RF[3]], writes=[self.RA[17 + c]])

    def conv_silu(self):
        P = self.P
        for c in range(4):
            P.emit("act", lambda e, o=self.hk(4 + c), y=self.ysq(c), sc=self.spc(C_LNG + c), bc=self.spc(C_LNB + c):
                   e.activation(out=o, in_=y, func=AF.Silu, scale=sc, bias=bc),
                   reads=[self.RA[17 + c], self.Rc], writes=[self.RH[4 + c]])
        self.preload_ln()

    def w_out(self, s, t):
        P = self.P
        self.norm_begin()
        pend = None
        for hf in range(2):
            sl, Rsl = self.wnext(("wout", hf))
            slv = sl[:].rearrange("p (m kc c) -> p m kc c", m=4, kc=8)
            for ml in range(4):
                mo = hf * 4 + ml
                b = self.bank()
                for kc in range(8):
                    self.mm(self.pb[b][:], slv[:, ml, kc, :], self.hk(kc), kc == 0, kc == 7,
                            [Rsl, self.RH[kc]], [self.Rpb[b]])
                P.emit("dve", lambda e, o=self.xk(mo), i0=self.pb[b][:]: e.tensor_tensor(out=o, in0=i0, in1=o, op=ALU.add),
                       reads=[self.Rpb[b], self.Rx[mo]], writes=[self.Rx[mo]])
                if pend is not None:
                    self.norm_chunk(pend)
                pend = mo
            self.wdone()
        self.norm_chunk(pend)

    def tile(self, s, t):
        P = self.P
        if s == 0 and t == 0:
            self.prefetch_x(0, 0)
        self.load_x(s, t)
        self.ffn(0)
        self.norm_finish()
        P.dma("pool", "qa", lambda e: e.dma_start(out=self.A[64:84, 0:4096], in_=self.qaux_d[t, :, :]),
              writes=self.RA[0:8])
        self.qk_chunks(s, t, "q")
        self.qk_chunks(s, t, "k")
        self.v_chunks(s, t)
        self.conv_in(s, t)
        self.kmean(s, t)
        self.conv_mm(0)
        self.conv_mm(1)
        self.gate_scores(s, t)
        self.conv_mm(2)
        self.conv_mm(3)
        self.gate_rows(s, t)
        self.attention(s, t, between={0: self.conv_stats, 1: self.conv_rstd, 2: self.conv_apply, 6: self.conv_silu})
        self.w_out(s, t)
        nxt = (s, t + 1) if t + 1 < self.NT else ((s + 1, 0) if s + 1 < self.NSEQ else None)
        self.ffn(1, after_up=(lambda: self.prefetch_x(*nxt)) if nxt else None)
        self.store_x(s, t)
        if s == 0 and t == 0:
            assert self.p0_cast == self.NSLAB
            vv = self.Vc[:].rearrange("p (c a s d) -> p c a s d", c=32, a=4, s=3, d=64)
            for c in range(10, 32):
                P.emit("pool", lambda e, o=vv[:, c, :, 1, :]: e.memset(o, 1.0), writes=[self.RV] + self.Rst)


def _bf(a):
    return np.asarray(a, dtype=np.float32).astype(ml_dtypes.bfloat16)


def make_consts():
    identf = np.eye(128, dtype=np.float32)
    cb = np.zeros((128, 512), np.float32)
    cb[:, 0:128] = np.eye(128)
    cb[:, 128:256] = 1.0
    cb[0:64, 256:320] = 1.0
    cb[64:128, 320:384] = 1.0
    kk = np.arange(128)[:, None]
    qq = np.arange(128)[None, :]
    cb[:, 384:512] = np.where(kk <= qq, 0.0, NEGB)
    kaux = np.zeros((20, 4096), np.float32)
    key = np.arange(4096)
    for n in range(16):
        kaux[n] = (key // 256 == n)
    kaux[16] = 1.0
    kaux[17] = 1.0
    kaux[18] = key % 256
    kaux[19] = 256 * (key // 256)
    slopes = 2.0 ** (-8.0 * (np.arange(8) + 1.0) / 8)
    qaux = np.zeros((8, 20, 8, 512), np.float32)
    it = np.arange(512)
    for t in range(8):
        bq = 2 * t + it // 256
        for n in range(16):
            qaux[t, n, :, :] = np.where(n <= bq, 0.0, NEGB)[None, :]
        for h in range(8):
            qaux[t, 16, h] = -slopes[h] * (it % 256)
            qaux[t, 17, h] = -slopes[h] * 256 * bq
            qaux[t, 18, h] = slopes[h]
            qaux[t, 19, h] = slopes[h]
    return identf, _bf(cb), _bf(kaux), _bf(qaux.reshape(8, 20, 4096))


def make_sp(inp):
    sp = np.zeros((128, NSP), np.float32)
    sp[:, C_G1:C_G1 + 8] = inp["ffn1_norm"][0].reshape(8, 128).T
    sp[:, C_GM:C_GM + 8] = inp["mix_norm"][0].reshape(8, 128).T
    sp[:, C_G2:C_G2 + 8] = inp["ffn2_norm"][0].reshape(8, 128).T
    sp[:, C_GQ] = np.tile(inp["q_norm"][0], 2)
    sp[:, C_GK] = np.tile(inp["k_norm"][0], 2)
    sp[:, C_DWB:C_DWB + 4] = inp["conv_dw_b"][0].reshape(4, 128).T
    sp[:, C_LNG:C_LNG + 4] = inp["conv_ln_g"][0].reshape(4, 128).T
    sp[:, C_LNB:C_LNB + 4] = inp["conv_ln_b"][0].reshape(4, 128).T
    w = inp["conv_dw_w"][0].reshape(31, 4, 128)
    sp[:, C_DWW:C_DWW + 124] = w.transpose(2, 1, 0).reshape(128, 124)
    return sp


_NC_CACHE = {}


def run(inp, n_cores=8, NSEQ=2, NT=8):
    key = (NSEQ, NT)
    if key not in _NC_CACHE:
        _NC_CACHE[key] = MK(NSEQ, NT).build()
    nc = _NC_CACHE[key]
    identf, cbf, kaux, qaux = make_consts()
    sp = make_sp(inp)
    f32 = lambda a: np.ascontiguousarray(a, dtype=np.float32)
    shared = {
        "f0w1": f32(inp["ffn1_w1"][0]), "f0w3": f32(inp["ffn1_w3"][0]), "f0w2": f32(inp["ffn1_w2"][0]),
        "f1w1": f32(inp["ffn2_w1"][0]), "f1w3": f32(inp["ffn2_w3"][0]), "f1w2": f32(inp["ffn2_w2"][0]),
        "win": f32(inp["w_in"][0]), "wout": f32(inp["w_out"][0]),
        "sp": sp, "identf": identf, "cbf": cbf, "kaux": kaux, "qaux": qaux,
    }
    S = NT * T
    x = inp["x"]
    in_maps = []
    for c in range(n_cores):
        m = dict(shared)
        m["x"] = f32(x[c * NSEQ:(c + 1) * NSEQ, :S, :])
        in_maps.append(m)
    res = run_bass_kernel_spmd(nc, in_maps, core_ids=list(range(n_cores)))
    return np.concatenate([r["out"] for r in res.results], axis=0)


def kernel(**inputs):
    inp = {k: np.asarray(v) for k, v in inputs.items()}
    out = run(inp, n_cores=8, NSEQ=2, NT=8)
    return out.astype(np.float32)
```

```python
import numpy as np
import ml_dtypes
from contextlib import ExitStack
import concourse.bass as bass
import concourse.mybir as mybir
from concourse.bass_utils import run_bass_kernel_spmd

F32 = mybir.dt.float32
BF16 = mybir.dt.bfloat16
AF = mybir.ActivationFunctionType
ALU = mybir.AluOpType
AX = mybir.AxisListType

D = 1024
DFF = 2816
NM = 22
T = 512
NEGB = -30000.0
EPS = 1e-6
NSLOT = 3
ENGS = ["pe", "act", "dve", "pool", "sp"]

C_G1, C_GM, C_G2, C_GQ, C_GK, C_DWB, C_LNG, C_LNB, C_DWW = 0, 8, 16, 24, 25, 26, 30, 34, 38
NSP = C_DWW + 124


class Res:
    __slots__ = ("name", "lw", "rd")

    def __init__(self, name):
        self.name = name
        self.lw = None
        self.rd = []


class Prog:
    def __init__(self, nc):
        self.nc = nc
        self.q = {e: [] for e in ENGS}
        self.tick = {e: 0 for e in ENGS}
        self.waited = {e: {} for e in ENGS}
        self.sems = {}
        self.dmacnt = {}

    def add_sem(self, name, handle):
        self.sems[name] = handle
        if name not in ENGS:
            self.dmacnt[name] = 0

    def _deps(self, eng, reads, writes):
        deps = {}

        def add(cv):
            if cv is None:
                return
            c, v = cv
            if deps.get(c, 0) < v:
                deps[c] = v
        for r in reads:
            add(r.lw)
        for w in writes:
            add(w.lw)
            for x in w.rd:
                add(x)
        out = []
        for c, v in deps.items():
            if c == eng and eng == "pe":
                continue
            if c in self.tick:
                assert v <= self.tick[c], ("wait on unsignalled tick", eng, c, v, self.tick[c])
            if self.waited[eng].get(c, 0) >= v:
                continue
            self.waited[eng][c] = v
            out.append((c, v))
        return out

    def _record(self, cv, reads, writes):
        for r in reads:
            r.rd.append(cv)
            if len(r.rd) > 64:
                m = {}
                for c, v in r.rd:
                    if m.get(c, 0) < v:
                        m[c] = v
                r.rd = list(m.items())
        for w in writes:
            w.lw = cv
            w.rd = []

    def emit(self, eng, fn, reads=(), writes=(), signal=True):
        waits = self._deps(eng, reads, writes)
        for c, v in waits:
            if c == eng:
                assert v <= self.tick[eng], (eng, v, self.tick[eng])
        if signal:
            self.tick[eng] += 1
            mytick = self.tick[eng]
        else:
            mytick = self.tick[eng] + 1
        sem = self.sems[eng]
        sems = self.sems

        def thunk(e):
            for c, v in waits:
                e.wait_ge(sems[c], v)
            ins = fn(e)
            if signal:
                ins.then_inc(sem, 1)
        self.q[eng].append(thunk)
        cv = (eng, mytick)
        self._record(cv, reads, writes)
        return cv

    def dma(self, eng, dsem, fn, reads=(), writes=()):
        waits = self._deps(eng, reads, writes)
        self.dmacnt[dsem] += 16
        val = self.dmacnt[dsem]
        sems = self.sems

        def thunk(e):
            for c, v in waits:
                e.wait_ge(sems[c], v)
            fn(e).then_inc(sems[dsem], 16)
        self.q[eng].append(thunk)
        cv = (dsem, val)
        self._record(cv, reads, writes)
        return cv

    def barrier(self):
        sems = self.sems
        for eng in ENGS:
            waits = []
            for c in ENGS:
                if c != eng and self.tick[c] > self.waited[eng].get(c, 0):
                    waits.append((c, self.tick[c]))
                    self.waited[eng][c] = self.tick[c]
            for c, v in self.dmacnt.items():
                if v > self.waited[eng].get(c, 0):
                    waits.append((c, v))
                    self.waited[eng][c] = v

            def thunk(e, waits=waits):
                for c, v in waits:
                    e.wait_ge(sems[c], v)
            self.q[eng].append(thunk)

    def wait_all(self, eng, ress):
        waits = self._deps(eng, (), ress)
        sems = self.sems

        def thunk(e):
            for c, v in waits:
                e.wait_ge(sems[c], v)
        self.q[eng].append(thunk)

    def run(self, block):
        q = self.q

        @block.tensor
        def _(e):
            for t in q["pe"]:
                t(e)

        @block.scalar
        def _(e):
            for t in q["act"]:
                t(e)

        @block.vector
        def _(e):
            for t in q["dve"]:
                t(e)

        @block.gpsimd
        def _(e):
            for t in q["pool"]:
                t(e)

        @block.sync
        def _(e):
            for t in q["sp"]:
                t(e)


def slab_descs():
    d = []
    for f in (0,):
        pass
    def ffn(f):
        for i in range(11):
            d.append(("up", f, i))
        for hf in range(2):
            for kg in range(3):
                d.append(("down", f, hf, kg))
    ffn(0)
    for nm in ("q", "k", "v", "za", "zg"):
        d.append(("win", nm))
    for c in range(4):
        d.append(("conv", c))
    for hf in range(2):
        d.append(("wout", hf))
    ffn(1)
    return d


class MK:
    def __init__(self, NSEQ, NT):
        self.NSEQ, self.NT = NSEQ, NT
        self.S = NT * T
        self.descs = slab_descs()
        self.NSLAB = len(self.descs)

    def build(self):
        nc = self.nc = bass.Bass("TRN2", target_bir_lowering=False)
        NSEQ, S = self.NSEQ, self.S
        dt = nc.dram_tensor
        self.x_d = dt("x", [NSEQ, S, D], F32, kind="ExternalInput").ap()
        self.w_d = {}
        for f in (0, 1):
            self.w_d[("w1", f)] = dt(f"f{f}w1", [D, DFF], F32, kind="ExternalInput").ap()
            self.w_d[("w3", f)] = dt(f"f{f}w3", [D, DFF], F32, kind="ExternalInput").ap()
            self.w_d[("w2", f)] = dt(f"f{f}w2", [DFF, D], F32, kind="ExternalInput").ap()
        self.win_d = dt("win", [D, 2560], F32, kind="ExternalInput").ap()
        self.wout_d = dt("wout", [D, D], F32, kind="ExternalInput").ap()
        self.sp_d = dt("sp", [128, NSP], F32, kind="ExternalInput").ap()
        self.idf_d = dt("identf", [128, 128], F32, kind="ExternalInput").ap()
        self.cbf_d = dt("cbf", [128, 512], BF16, kind="ExternalInput").ap()
        self.kaux_d = dt("kaux", [20, 4096], BF16, kind="ExternalInput").ap()
        self.qaux_d = dt("qaux", [8, 20, 4096], BF16, kind="ExternalInput").ap()
        self.out_d = dt("out", [NSEQ, S, D], F32, kind="ExternalOutput").ap()
        self.ws_d = dt("wstream", [self.NSLAB, 128, 4096], BF16).ap()

        with ExitStack() as es:
            def sb(name, shape, dtp):
                return es.enter_context(nc.sbuf_tensor(name, shape, dtp))

            def ps(name, shape, dtp):
                return es.enter_context(nc.psum_tensor(name, shape, dtp))
            self.xfm = sb("xfm", [128, 4096], F32)
            self.H = sb("H", [128, 2048], F32)
            self.A = sb("A", [128, NM * 512], BF16)
            self.Kc = [sb(f"Kc{h}", [84, 4096], BF16) for h in range(8)]
            self.Vc = sb("Vc", [128, 32 * 768], BF16)
            self.PT = sb("PT", [128, 6 * 512], BF16)
            self.Fall = sb("Fall", [128, 2048], F32)
            self.Fs = [self.Fall[:, i * 512:(i + 1) * 512] for i in range(4)]
            self.SQ = sb("SQ", [128, 2 * 512], BF16)
            self.ring = [sb(f"ring{i}", [128, 4096], BF16) for i in range(NSLOT)]
            self.spt = sb("spt", [128, NSP], F32)
            self.identf = sb("identf_sb", [128, 128], F32)
            self.cbf = sb("cbf_sb", [128, 512], BF16)
            self.cst = sb("cst", [128, 4], F32)
            self.kmT = sb("kmT", [64, 128], BF16)
            self.kmf = sb("kmf", [128, 2], F32)
            self.gsb = sb("gsb", [128, 128], F32)
            self.top8 = sb("top8", [128, 64], F32)
            self.msk = sb("msk", [128, 128], F32)
            self.mbw = sb("mbw", [128, 4 * 192], BF16)
            self.halo = sb("halo", [128, 4 * 30], BF16)
            self.pb = [ps(f"pb{i}", [128, 512], F32) for i in range(8)]

            P = self.P = Prog(nc)
            for e in ENGS:
                P.add_sem(e, es.enter_context(nc.semaphore("s_" + e)))
            dsems = ["cst", "ka", "qa", "st0", "st1"] + [f"xl{i}" for i in range(4)] + [f"xs{i}" for i in range(4)] + \
                [f"wl{i}" for i in range(NSLOT)] + [f"ws{i}" for i in range(NSLOT)]
            for dname in dsems:
                P.add_sem(dname, es.enter_context(nc.semaphore("d_" + dname)))
            block = es.enter_context(nc.Block())

            self.Rx = [Res(f"x{k}") for k in range(8)]
            self.RH = [Res(f"H{k}") for k in range(8)]
            self.RA = [Res(f"A{k}") for k in range(NM)]
            self.RK = [Res(f"K{h}") for h in range(8)]
            self.RV = Res("V")
            self.RPT = [Res(f"PT{i}") for i in range(6)]
            self.RF = [Res(f"F{i}") for i in range(4)]
            self.RSQ = [Res(f"SQ{i}") for i in range(2)]
            self.Rring = [Res(f"ring{i}") for i in range(NSLOT)]
            self.Rc = Res("consts")
            self.Rcst = Res("cst")
            self.Rcst2 = Res("cst2")
            self.Rcst3 = Res("cst3")
            self.Rkm = Res("kmT")
            self.Rkmf = Res("kmf")
            self.Rgsb = Res("gsb")
            self.Rtop = Res("top8")
            self.Rmsk = Res("msk")
            self.Rmbw = [Res(f"mbw{i}") for i in range(4)]
            self.Rpb = [Res(f"pb{i}") for i in range(8)]
            self.Rws = Res("wstream")
            self.Rslab = [Res(f"slab{i}") for i in range(self.NSLAB)]
            self.Rhalo = Res("halo")
            self.Rout = Res("out")
            self.rr = 0
            self.ptr = 0

            self.Hb = self.H[:].bitcast(BF16)
            self.ident_bf = self.cbf[:, 0:128]
            self.ones_bf = self.cbf[:, 128:256]
            self.bones_bf = self.cbf[:, 256:384]
            self.tri_bf = self.cbf[:, 384:512]

            self.phase0()
            self.init_main()
            self.wseq = [i for _ in range(NSEQ * self.NT) for i in range(self.NSLAB)]
            self.wpos = 0
            for s in range(NSEQ):
                self.seq_init(s)
                for t in range(self.NT):
                    self.tile(s, t)
            P.barrier()
            P.run(block)
        return nc

    def xk(self, k):
        return self.xfm[:, k * 512:(k + 1) * 512]

    def hk(self, k):
        return self.Hb[:, k * 512:(k + 1) * 512]

    def ak(self, m):
        return self.A[:, m * 512:(m + 1) * 512]

    def qa(self, h, rows=84):
        return self.A[0:rows, h * 512:(h + 1) * 512]

    def ut(self, c):
        return self.A[:, 4096 + c * 542: 4096 + (c + 1) * 542]

    R_UT = property(lambda self: self.RA[8:13])

    def ybf(self, c):
        return self.ak(13 + c)

    def ysq(self, c):
        return self.ak(17 + c)

    def bank(self):
        i = self.rr
        self.rr = (self.rr + 1) % 6
        return i

    def spc(self, c, lo=0, hi=128):
        return self.spt[lo:hi, c:c + 1]

    def eng_alt(self):
        self._alt = getattr(self, "_alt", 0) ^ 1
        return "act" if self._alt else "dve"

    def copy_any(self, eng, out, in_, reads, writes):
        if eng == "act":
            self.P.emit("act", lambda e: e.activation(out=out, in_=in_, func=AF.Copy), reads, writes)
        else:
            self.P.emit(eng, lambda e: e.tensor_copy(out=out, in_=in_), reads, writes)

    def scale_cast(self, eng, out, in_, col, reads, writes):
        if eng == "act":
            self.P.emit("act", lambda e: e.activation(out=out, in_=in_, func=AF.Copy, scale=col), reads, writes)
        else:
            self.P.emit("dve", lambda e: e.tensor_scalar(out=out, in0=in_, scalar1=col, scalar2=None, op0=ALU.mult),
                        reads, writes)

    def phase0(self):
        P = self.P
        P.dma("sp", "cst", lambda e: e.dma_start(out=self.spt[:], in_=self.sp_d[:, :]), writes=[self.Rc])
        P.dma("sp", "cst", lambda e: e.dma_start(out=self.identf[:], in_=self.idf_d[:, :]), writes=[self.Rc])
        P.dma("sp", "cst", lambda e: e.dma_start(out=self.cbf[:], in_=self.cbf_d[:, :]), writes=[self.Rc])
        vf = self.Vc[:].bitcast(F32)
        stg = [vf[:, 4096:8192], vf[:, 8192:12288]]
        Rst = self.Rst = [Res("stg0"), Res("stg1")]
        par, k = [], 0
        for desc in self.descs:
            par.append(k)
            if desc[0] != "conv":
                k ^= 1

        def loads(idx):
            desc = self.descs[idx]
            kk_ = par[idx]
            st, Rs, ds = stg[kk_], Rst[kk_], f"st{kk_}"
            kind = desc[0]
            if kind == "up":
                f, i = desc[1], desc[2]
                stv = st.rearrange("p (w kc n) -> p w kc n", w=2, kc=8, n=256)
                for w, nm in enumerate(("w1", "w3")):
                    src = self.w_d[(nm, f)].rearrange("(kc p) n -> p kc n", p=128)[:, :, 256 * i:256 * i + 256]
                    P.dma("sp", ds, lambda e, o=stv[:, w], s_=src: e.dma_start(out=o, in_=s_), writes=[Rs])
            elif kind == "down":
                f, hf, kg = desc[1], desc[2], desc[3]
                nk = min(8, NM - 8 * kg)
                src = self.w_d[("w2", f)].rearrange("(kc p) n -> p kc n", p=128)[:, 8 * kg:8 * kg + nk, 512 * hf:512 * hf + 512]
                stv = st[:, 0:nk * 512].rearrange("p (kc c) -> p kc c", kc=nk)
                P.dma("sp", ds, lambda e, o=stv, s_=src: e.dma_start(out=o, in_=s_), writes=[Rs])
            elif kind == "win":
                c0 = {"q": 0, "k": 512, "v": 1024, "za": 1536, "zg": 2048}[desc[1]]
                src = self.win_d.rearrange("(kc p) n -> p kc n", p=128)[:, :, c0:c0 + 512]
                stv = st.rearrange("p (kc n) -> p kc n", kc=8)
                P.dma("sp", ds, lambda e, o=stv, s_=src: e.dma_start(out=o, in_=s_), writes=[Rs])
            elif kind == "wout":
                hf = desc[1]
                src = self.wout_d.rearrange("(kc p) n -> p kc n", p=128)[:, :, hf * 512:hf * 512 + 512]
                stv = st.rearrange("p (kc n) -> p kc n", kc=8)
                P.dma("sp", ds, lambda e, o=stv, s_=src: e.dma_start(out=o, in_=s_), writes=[Rs])

        def casts(idx):
            desc = self.descs[idx]
            slot = idx % NSLOT
            sl = self.ring[slot][:]
            Rsl = self.Rring[slot]
            kk_ = par[idx]
            st, Rs = stg[kk_], Rst[kk_]
            kind = desc[0]
            if kind == "up":
                f, i = desc[1], desc[2]
                gc = C_G1 if f == 0 else C_G2
                stv = st.rearrange("p (w kc n) -> p w kc n", w=2, kc=8, n=256)
                slv = sl.rearrange("p (j w kc c) -> p j w kc c", j=2, w=2, kc=8, c=128)
                for w in range(2):
                    for kc in range(8):
                        self.scale_cast(self.eng_alt(), slv[:, :, w, kc, :],
                                        stv[:, w, kc, :].rearrange("p (j c) -> p j c", j=2),
                                        self.spc(gc + kc), [Rs, self.Rc], [Rsl])
            elif kind == "down":
                nk = min(8, NM - 8 * desc[3])
                self.copy_any(self.eng_alt(), sl[:, 0:nk * 512], st[:, 0:nk * 512], [Rs], [Rsl])
            elif kind == "win":
                nm = desc[1]
                stv = st.rearrange("p (kc n) -> p kc n", kc=8)
                for kc in range(8):
                    if nm == "v":
                        o = sl[:, kc * 512:(kc + 1) * 512]
                        i_ = stv[:, kc, :]
                    else:
                        o = sl.rearrange("p (m kc c) -> p m kc c", m=4, kc=8)[:, :, kc, :]
                        i_ = stv[:, kc, :].rearrange("p (m c) -> p m c", m=4)
                    self.scale_cast(self.eng_alt(), o, i_, self.spc(C_GM + kc), [Rs, self.Rc], [Rsl])
            elif kind == "conv":
                c = desc[1]
                for kk in range(31):
                    self.scale_cast(self.eng_alt(), sl[:, kk * 128:(kk + 1) * 128], self.identf[:],
                                    self.spc(C_DWW + c * 31 + kk), [self.Rc], [Rsl])
            elif kind == "wout":
                stv = st.rearrange("p (kc n) -> p kc n", kc=8)
                for kc in range(8):
                    o = sl.rearrange("p (m kc c) -> p m kc c", m=4, kc=8)[:, :, kc, :]
                    i_ = stv[:, kc, :].rearrange("p (m c) -> p m c", m=4)
                    self.copy_any(self.eng_alt(), o, i_, [Rs], [Rsl])
            P.dma("pool", f"ws{slot}", lambda e, o=self.ws_d[idx, :, :], s_=sl: e.dma_start(out=o, in_=s_),
                  reads=[Rsl], writes=[self.Rslab[idx]])

        self._p0_loads, self._p0_casts = loads, casts
        self.p0_loaded = 0
        self.p0_cast = 0

    def p0_advance(self, upto):
        n = self.NSLAB
        upto = min(upto, n)
        while self.p0_cast < upto:
            while self.p0_loaded < min(self.p0_cast + 2, n):
                self._p0_loads(self.p0_loaded)
                self.p0_loaded += 1
            self._p0_casts(self.p0_cast)
            self.p0_cast += 1

    def init_main(self):
        P = self.P
        vv = self.Vc[:].rearrange("p (c a s d) -> p c a s d", c=32, a=4, s=3, d=64)
        for c in range(10):
            P.emit("pool", lambda e, o=vv[:, c, :, 1, :]: e.memset(o, 1.0), writes=[self.RV])
        for h in range(8):
            P.dma("sp", "ka", lambda e, h=h: e.dma_start(out=self.Kc[h][64:84, :], in_=self.kaux_d[:, :]),
                  writes=[self.RK[h]])
        for h in range(8):
            self.RK[h].lw = ("ka", P.dmacnt["ka"])
        P.emit("pool", lambda e: e.memset(self.mbw[:], 0.0), writes=self.Rmbw)
        P.emit("dve", lambda e: e.memset(self.cst[:, 0:1], EPS), writes=[self.Rcst])
        P.emit("dve", lambda e: e.memset(self.cst[:, 2:3], 1.0), writes=[self.Rcst2])
        P.emit("dve", lambda e: e.tensor_scalar(out=self.cst[:, 1:2], in0=self.spc(C_GQ), scalar1=0.125,
                                                scalar2=None, op0=ALU.mult), reads=[self.Rc], writes=[self.Rcst])

    def seq_init(self, s):
        P = self.P
        P.emit("pool", lambda e: e.memset(self.halo[:], 0.0), writes=[self.Rhalo])
        P.emit("pool", lambda e: e.memset(self.gsb[:], NEGB), writes=[self.Rgsb])
        P.emit("pool", lambda e: e.memset(self.kmT[:], 0.0), writes=[self.Rkm])

    def _wload(self, j):
        if j < self.NSLAB:
            return
        slot = j % NSLOT
        idx = self.wseq[j]
        self.P.dma("sp", f"wl{slot}", lambda e: e.dma_start(out=self.ring[slot][:], in_=self.ws_d[idx, :, :]),
                   reads=[self.Rslab[idx]], writes=[self.Rring[slot]])

    def wnext(self, expect):
        j = self.wpos
        assert self.descs[self.wseq[j]] == expect, (self.descs[self.wseq[j]], expect)
        if j < self.NSLAB:
            self.p0_advance(j + NSLOT)
        slot = j % NSLOT
        return self.ring[slot], self.Rring[slot]

    def wdone(self):
        j = self.wpos
        self.wpos += 1
        if j + NSLOT < len(self.wseq):
            self._wload(j + NSLOT)

    def mm(self, out, lhsT, rhs, start, stop, reads, writes, signal=None):
        if signal is None:
            signal = stop
        self.P.emit("pe", lambda e: e.matmul(out, lhsT=lhsT, rhs=rhs, start=start, stop=stop),
                    reads, writes, signal)

    def stg_in(self, i):
        if i < 2:
            return self.H[:, i * 1024:(i + 1) * 1024], self.RH[4 * i:4 * i + 4]
        j = i - 2
        return self.Fall[:, j * 1024:(j + 1) * 1024], self.RF[2 * j:2 * j + 2]

    def stg_out(self, i):
        return self.A[:].bitcast(F32)[:, i * 1024:(i + 1) * 1024], self.RA[4 * i:4 * i + 4]

    def prefetch_x(self, s, t):
        for tc in range(4):
            st, Rst = self.stg_in(tc)
            r0 = t * T + tc * 128
            self.P.dma("pool", f"xl{tc}", lambda e, o=st, s_=self.x_d[s, r0:r0 + 128, :]: e.dma_start(out=o, in_=s_),
                       writes=Rst)

    def load_x(self, s, t):
        P = self.P
        self.norm_begin()
        self.preload_ln()
        pend = None
        for kc in range(8):
            b = self.bank()
            for tc in range(4):
                st, Rst = self.stg_in(tc)
                P.emit("pe", lambda e, o=self.pb[b][:, tc * 128:(tc + 1) * 128], i_=st[:, kc * 128:(kc + 1) * 128]:
                       e.transpose(out=o, in_=i_, identity=self.identf[:]),
                       reads=Rst + [self.Rc], writes=[self.Rpb[b]], signal=(tc == 3))
            self.copy_any("dve", self.xk(kc), self.pb[b][:], [self.Rpb[b]], [self.Rx[kc]])
            if pend is not None:
                self.norm_chunk(pend)
            pend = kc
        self.norm_chunk(pend)

    def store_x(self, s, t):
        P = self.P
        for tc in range(4):
            st, Rst = self.stg_out(tc)
            r0 = t * T + tc * 128
            for g in range(2):
                b = self.bank()
                for kl in range(4):
                    kc = g * 4 + kl
                    P.emit("pe", lambda e, o=self.pb[b][:, kl * 128:(kl + 1) * 128],
                           i_=self.xk(kc)[:, tc * 128:(tc + 1) * 128]:
                           e.transpose(out=o, in_=i_, identity=self.identf[:]),
                           reads=[self.Rx[kc], self.Rc], writes=[self.Rpb[b]], signal=(kl == 3))
                self.copy_any(self.eng_alt(), st[:, g * 512:(g + 1) * 512], self.pb[b][:], [self.Rpb[b]], Rst)
            P.dma("pool", f"xs{tc}", lambda e, o=self.out_d[s, r0:r0 + 128, :], s_=st: e.dma_start(out=o, in_=s_),
                  reads=Rst, writes=[self.Rout])

    def rstd_from(self, bnk, inv_n, Fi):
        P = self.P
        F, RF = self.Fs[Fi], self.RF[Fi]
        P.emit("act", lambda e: e.activation(out=F[:], in_=self.pb[bnk][:], func=AF.Ln, scale=inv_n,
                                             bias=self.cst[:, 0:1]),
               reads=[self.Rpb[bnk], self.Rcst], writes=[RF])
        P.emit("act", lambda e: e.activation(out=F[:], in_=F[:], func=AF.Exp, scale=-0.5), reads=[RF], writes=[RF])

    def preload_ln(self):
        self.P.emit("act", lambda e: e.activation(out=self.cst[:, 3:4], in_=self.cst[:, 2:3], func=AF.Ln),
                    reads=[self.Rcst2], writes=[self.Rcst3])

    def norm_begin(self):
        self.ncount = 0

    def norm_chunk(self, kc, from_bank=None):
        P = self.P
        i = self.ncount % 2
        if from_bank is None:
            src, Rsrc = self.xk(kc), self.Rx[kc]
        else:
            src, Rsrc = self.pb[from_bank][:], self.Rpb[from_bank]
        P.emit("act", lambda e, o=self.SQ[:, i * 512:(i + 1) * 512], i_=src: e.activation(out=o, in_=i_, func=AF.Square),
               reads=[Rsrc], writes=[self.RSQ[i]])
        self.mm(self.pb[6][:], self.ones_bf, self.SQ[:, i * 512:(i + 1) * 512], self.ncount == 0, self.ncount == 7,
                [self.RSQ[i], self.Rc], [self.Rpb[6]], signal=True)
        self.ncount += 1

    def norm_finish(self):
        P = self.P
        assert self.ncount == 8
        self.rstd_from(6, 1.0 / D, 3)
        for kc in range(8):
            eng = "pool" if kc in (2, 4, 6) else "dve"
            P.emit(eng, lambda e, o=self.hk(kc), i_=self.xk(kc): e.tensor_tensor(out=o, in0=i_, in1=self.Fs[3][:], op=ALU.mult),
                   reads=[self.Rx[kc], self.RF[3]], writes=[self.RH[kc]])

    def ffn(self, f, after_up=None):
        P = self.P
        self.norm_finish()
        for i in range(11):
            sl, Rsl = self.wnext(("up", f, i))
            slv = sl[:].rearrange("p (j w kc c) -> p j w kc c", j=2, w=2, kc=8, c=128)
            pre = None
            if i == 0:
                pre = [(self.bank(), self.bank()) for _ in range(2)]
                for kc in range(8):
                    for j in range(2):
                        for w in range(2):
                            bk = pre[j][w]
                            self.mm(self.pb[bk][:], slv[:, j, w, kc, :], self.hk(kc), kc == 0, kc == 7,
                                    [Rsl, self.RH[kc]], [self.Rpb[bk]])
            for j in range(2):
                m = 2 * i + j
                if pre is not None:
                    ba, bb = pre[j]
                else:
                    ba, bb = self.bank(), self.bank()
                    for w, bk in ((0, ba), (1, bb)):
                        for kc in range(8):
                            self.mm(self.pb[bk][:], slv[:, j, w, kc, :], self.hk(kc), kc == 0, kc == 7,
                                    [Rsl, self.RH[kc]], [self.Rpb[bk]])
                Fi = m % 2
                P.emit("act", lambda e, o=self.Fs[Fi][:], i_=self.pb[ba][:]: e.activation(out=o, in_=i_, func=AF.Silu),
                       reads=[self.Rpb[ba]], writes=[self.RF[Fi]])
                P.emit("dve", lambda e, o=self.ak(m), i0=self.pb[bb][:], i1=self.Fs[Fi][:]:
                       e.tensor_tensor(out=o, in0=i0, in1=i1, op=ALU.mult),
                       reads=[self.Rpb[bb], self.RF[Fi]], writes=[self.RA[m]])
            self.wdone()
        if after_up is not None:
            after_up()
        if f == 0:
            self.norm_begin()
            self.preload_ln()
        pend = []
        for hf in range(2):
            banks = [self.bank() for _ in range(4)]
            for kg in range(3):
                sl, Rsl = self.wnext(("down", f, hf, kg))
                nk = min(8, NM - 8 * kg)
                for ml in range(4):
                    bk = banks[ml]
                    for kl in range(nk):
                        kc = 8 * kg + kl
                        self.mm(self.pb[bk][:], sl[:, kl * 512 + ml * 128:kl * 512 + (ml + 1) * 128], self.ak(kc),
                                kc == 0, kc == NM - 1, [Rsl, self.RA[kc]], [self.Rpb[bk]],
                                signal=(kc == NM - 1) or (ml == 3 and kl == nk - 1))
                self.wdone()
            for ml in range(4):
                mo = hf * 4 + ml
                P.emit("dve", lambda e, o=self.xk(mo), i0=self.pb[banks[ml]][:]:
                       e.scalar_tensor_tensor(out=o, in0=i0, scalar=0.5, in1=o, op0=ALU.mult, op1=ALU.add),
                       reads=[self.Rpb[banks[ml]], self.Rx[mo]], writes=[self.Rx[mo]])
            if f == 0:
                for mo in pend:
                    self.norm_chunk(mo)
                pend = [hf * 4 + ml for ml in range(4)]
        if f == 0:
            for mo in pend:
                self.norm_chunk(mo)

    def qk_chunks(self, s, t, which):
        P = self.P
        sl, Rsl = self.wnext(("win", which))
        slv = sl[:].rearrange("p (m kc c) -> p m kc c", m=4, kc=8)
        col0 = t * T
        pend = None

        def finish(c, bz):
            i = c % 2
            bs = self.bank()
            self.mm(self.pb[bs][:], self.bones_bf, self.SQ[:, i * 512:(i + 1) * 512], True, True,
                    [self.RSQ[i], self.Rc], [self.Rpb[bs]])
            Fi = c % 2
            self.rstd_from(bs, 1.0 / 64, Fi)
            for hh in range(2):
                h = 2 * c + hh
                lo, hi = hh * 64, hh * 64 + 64
                if which == "q":
                    o, Ro = self.qa(h, 64), [self.RA[h]]
                    colap, Rcol = self.cst[lo:hi, 1:2], self.Rcst
                else:
                    o, Ro = self.Kc[h][0:64, col0:col0 + T], [self.RK[h]]
                    colap, Rcol = self.spc(C_GK, lo, hi), self.Rc
                P.emit("dve", lambda e, o=o, i0=self.pb[bz][lo:hi, :], i1=self.Fs[Fi][lo:hi, :], colap=colap:
                       e.scalar_tensor_tensor(out=o, in0=i0, scalar=colap, in1=i1, op0=ALU.mult, op1=ALU.mult),
                       reads=[self.Rpb[bz], self.RF[Fi], Rcol], writes=Ro)
        pre = {}
        if which == "q":
            pre = {0: self.bank(), 1: self.bank()}
            for kc in range(8):
                for c in (0, 1):
                    self.mm(self.pb[pre[c]][:], slv[:, c, kc, :], self.hk(kc), kc == 0, kc == 7,
                            [Rsl, self.RH[kc]], [self.Rpb[pre[c]]])
        for c in range(4):
            if c in pre:
                bz = pre[c]
            else:
                bz = self.bank()
                for kc in range(8):
                    self.mm(self.pb[bz][:], slv[:, c, kc, :], self.hk(kc), kc == 0, kc == 7,
                            [Rsl, self.RH[kc]], [self.Rpb[bz]])
            i = c % 2
            P.emit("act", lambda e, o=self.SQ[:, i * 512:(i + 1) * 512], i_=self.pb[bz][:]: e.activation(out=o, in_=i_, func=AF.Square),
                   reads=[self.Rpb[bz]], writes=[self.RSQ[i]])
            if pend is not None:
                finish(*pend)
            pend = (c, bz)
        self.wdone()
        finish(*pend)

    def v_chunks(self, s, t):
        sl, Rsl = self.wnext(("win", "v"))
        for tc in range(4):
            b = self.bank()
            for kc in range(8):
                self.mm(self.pb[b][:], self.hk(kc)[:, tc * 128:(tc + 1) * 128], sl[:, kc * 512:(kc + 1) * 512],
                        kc == 0, kc == 7, [Rsl, self.RH[kc]], [self.Rpb[b]])
            ch = t * 4 + tc
            o = self.Vc[:, ch * 768:(ch + 1) * 768].rearrange("p (a s d) -> p a s d", a=4, s=3, d=64)[:, :, 0:3:2, :]
            i_ = self.pb[b][:].rearrange("p (a s d) -> p a s d", a=4, s=2, d=64)
            self.copy_any(self.eng_alt(), o, i_, [self.Rpb[b]], [self.RV])
        self.wdone()

    def conv_in(self, s, t):
        P = self.P
        for c in range(4):
            P.emit("pool", lambda e, o=self.ut(c)[:, 0:30], i_=self.halo[:, c * 30:(c + 1) * 30]: e.tensor_copy(out=o, in_=i_),
                   reads=[self.Rhalo], writes=self.R_UT)
        sla, Rsla = self.wnext(("win", "za"))
        slav = sla[:].rearrange("p (m kc c) -> p m kc c", m=4, kc=8)
        banks = []
        for c in range(4):
            b = self.bank()
            for kc in range(8):
                self.mm(self.pb[b][:], slav[:, c, kc, :], self.hk(kc), kc == 0, kc == 7,
                        [Rsla, self.RH[kc]], [self.Rpb[b]])
            banks.append(b)
        self.wdone()
        slg, Rslg = self.wnext(("win", "zg"))
        slgv = slg[:].rearrange("p (m kc c) -> p m kc c", m=4, kc=8)
        for c in range(4):
            b = 6 + (c % 2)
            for kc in range(8):
                self.mm(self.pb[b][:], slgv[:, c, kc, :], self.hk(kc), kc == 0, kc == 7,
                        [Rslg, self.RH[kc]], [self.Rpb[b]])
            Fi = c % 2
            P.emit("act", lambda e, o=self.Fs[Fi][:], i_=self.pb[b][:]: e.activation(out=o, in_=i_, func=AF.Sigmoid),
                   reads=[self.Rpb[b]], writes=[self.RF[Fi]])
            P.emit("dve", lambda e, o=self.ut(c)[:, 30:542], i0=self.pb[banks[c]][:], i1=self.Fs[Fi][:]:
                   e.tensor_tensor(out=o, in0=i0, in1=i1, op=ALU.mult),
                   reads=[self.Rpb[banks[c]], self.RF[Fi]], writes=self.R_UT)
        self.wdone()

    def kmean(self, s, t):
        P = self.P
        col0 = t * T
        for h in range(8):
            kin = self.Kc[h][0:64, col0:col0 + T].rearrange("p (b k) -> p b k", b=2)
            P.emit("dve", lambda e, i_=kin: e.tensor_reduce(out=self.kmf[0:64, :], in_=i_, axis=AX.X, op=ALU.add),
                   reads=[self.RK[h]], writes=[self.Rkmf])
            P.emit("dve", lambda e, o=self.kmT[0:64, h * 16 + 2 * t:h * 16 + 2 * t + 2]:
                   e.tensor_scalar(out=o, in0=self.kmf[0:64, :], scalar1=1.0 / 256, scalar2=None, op0=ALU.mult),
                   reads=[self.Rkmf], writes=[self.Rkm])

    def gate_scores(self, s, t):
        P = self.P
        if t < 2:
            return
        for qc in range(4):
            b = 2 * t + qc // 2
            g = self.bank()
            for h in range(8):
                self.mm(self.pb[g][:, h * 16:(h + 1) * 16], self.qa(h, 64)[:, qc * 128:(qc + 1) * 128],
                        self.kmT[0:64, h * 16:(h + 1) * 16], True, True,
                        [self.RA[h], self.Rkm], [self.Rpb[g]], signal=(h == 7))
            gv = self.gsb[:].rearrange("p (h n) -> p h n", h=8)
            P.emit("dve", lambda e, o=gv[:, :, 0:b], i_=self.pb[g][:, 0:128].rearrange("p (h n) -> p h n", h=8)[:, :, 0:b]:
                   e.tensor_copy(out=o, in_=i_), reads=[self.Rpb[g]], writes=[self.Rgsb])
            for h in range(8):
                P.emit("dve", lambda e, o=self.top8[:, h * 8:(h + 1) * 8], i_=self.gsb[:, h * 16:(h + 1) * 16]:
                       e.max(out=o, in_=i_), reads=[self.Rgsb], writes=[self.Rtop])
            thr = self.top8[:].rearrange("p (h k) -> p h k", h=8)[:, :, 2:3].broadcast_to([128, 8, 16])
            P.emit("dve", lambda e, thr=thr: e.tensor_tensor(out=self.msk[:].rearrange("p (h n) -> p h n", h=8), in0=gv, in1=thr,
                                                            op=ALU.is_ge),
                   reads=[self.Rgsb, self.Rtop], writes=[self.Rmsk])
            mo = self.mbw[:, qc * 192 + 64:qc * 192 + 192]
            P.emit("dve", lambda e, mo=mo: e.tensor_scalar(out=mo, in0=self.msk[:], scalar1=1.0, scalar2=-NEGB,
                                                          op0=ALU.subtract, op1=ALU.mult),
                   reads=[self.Rmsk], writes=[self.Rmbw[qc]])
            P.emit("dve", lambda e, o=mo.rearrange("p (h n) -> p h n", h=8)[:, :, b:b + 1]: e.memset(o, 0.0),
                   writes=[self.Rmbw[qc]])

    def gate_rows(self, s, t):
        P = self.P
        if t < 2:
            return
        for h in range(8):
            g = self.bank()
            gb = self.pb[g][:].bitcast(BF16)
            for qc in range(4):
                P.emit("pe", lambda e, o=gb[0:80, qc * 128:(qc + 1) * 128],
                       i_=self.mbw[:, qc * 192 + 16 * h:qc * 192 + 16 * h + 80]:
                       e.transpose(out=o, in_=i_, identity=self.ident_bf),
                       reads=[self.Rmbw[qc], self.Rc], writes=[self.Rpb[g]], signal=(qc == 3))
            self.copy_any("dve", self.qa(h)[64:80, :], gb[64:80, 0:512], [self.Rpb[g]], [self.RA[h]])

    def attention(self, s, t, between=None):
        P = self.P
        b0, b1 = 2 * t, 2 * t + 1
        for h in range(8):
            acc = 6 + (h % 2)
            pair, odd = h // 2, h % 2
            vlo = pair * 192 + (64 if odd else 0)
            qh = self.qa(h)
            chunks = []
            for n in range(b0):
                for kc in range(2):
                    chunks.append((n * 256 + kc * 128, 0, 512, False))
            chunks.append((b0 * 256, 0, 512, True))
            chunks.append((b0 * 256 + 128, 128, 512, True))
            chunks.append((b1 * 256, 256, 512, True))
            chunks.append((b1 * 256 + 128, 384, 512, True))
            staged = []

            def qk(ci):
                k0, ql, qhi, tri = chunks[ci]
                n_ = qhi - ql
                sb_ = self.bank()
                self.mm(self.pb[sb_][:, 0:n_], self.Kc[h][0:84, k0:k0 + 128], qh[:, ql:qhi],
                        True, not tri, [self.RK[h], self.RA[h]], [self.Rpb[sb_]])
                if tri:
                    self.mm(self.pb[sb_][:, 0:128], self.ident_bf, self.tri_bf, False, True,
                            [self.Rc], [self.Rpb[sb_]])
                pi = self.ptr
                self.ptr = (self.ptr + 1) % 6
                P.emit("act", lambda e, o=self.PT[:, pi * 512:pi * 512 + n_], i_=self.pb[sb_][:, 0:n_]:
                       e.activation(out=o, in_=i_, func=AF.Exp),
                       reads=[self.Rpb[sb_]], writes=[self.RPT[pi]])
                staged.append((ci, pi))

            def pv(first, last):
                ci, pi = staged.pop(0)
                k0, ql, qhi, tri = chunks[ci]
                n_ = qhi - ql
                ch = k0 // 128
                self.mm(self.pb[acc][:, ql:qhi], self.Vc[:, ch * 768 + vlo:ch * 768 + vlo + 128],
                        self.PT[:, pi * 512:pi * 512 + n_], first, last,
                        [self.RV, self.RPT[pi]], [self.Rpb[acc]], signal=(last or ci + LOOK >= nch))
            nch = len(chunks)
            LOOK = 4
            for ci in range(min(LOOK, nch)):
                qk(ci)
            for ci in range(nch):
                pv(ci == 0, ci == nch - 1)
                if ci + LOOK < nch:
                    qk(ci + LOOK)
            alo, dlo = (64, 0) if odd else (0, 64)
            rec = self.Fs[2][alo:alo + 64, :]
            if h == 7:
                P.emit("act", lambda e, rec=rec, i_=self.pb[acc][dlo:dlo + 64, :]: e.activation(out=rec, in_=i_, func=AF.Ln),
                       reads=[self.Rpb[acc]], writes=[self.RF[2]])
                P.emit("act", lambda e, rec=rec: e.activation(out=rec, in_=rec, func=AF.Exp, scale=-1.0),
                       reads=[self.RF[2]], writes=[self.RF[2]])
            else:
                P.emit("dve", lambda e, rec=rec, i_=self.pb[acc][dlo:dlo + 64, :]: e.reciprocal(out=rec, in_=i_),
                       reads=[self.Rpb[acc]], writes=[self.RF[2]])
            o = self.hk(pair)[alo:alo + 64, :]
            P.emit("dve", lambda e, o=o, i0=self.pb[acc][alo:alo + 64, :], rec=rec:
                   e.tensor_tensor(out=o, in0=i0, in1=rec, op=ALU.mult),
                   reads=[self.Rpb[acc], self.RF[2]], writes=[self.RH[pair]])
            if between and h in between:
                between[h]()

    def conv_mm(self, c):
        P = self.P
        sl, Rsl = self.wnext(("conv", c))
        b = self.bank()
        for kk in range(31):
            self.mm(self.pb[b][:], sl[:, kk * 128:(kk + 1) * 128], self.ut(c)[:, kk:kk + 512], kk == 0, kk == 30,
                    [Rsl] + self.R_UT, [self.Rpb[b]])
        self.wdone()
        P.emit("dve", lambda e, o=self.ybf(c), i_=self.pb[b][:], bc=self.spc(C_DWB + c):
               e.tensor_scalar(out=o, in0=i_, scalar1=bc, scalar2=None, op0=ALU.add),
               reads=[self.Rpb[b], self.Rc], writes=[self.RA[13 + c]])
        P.emit("dve", lambda e, o=self.ysq(c), i_=self.pb[b][:], bc=self.spc(C_DWB + c), y=self.ybf(c):
               e.scalar_tensor_tensor(out=o, in0=i_, scalar=bc, in1=y, op0=ALU.add, op1=ALU.mult),
               reads=[self.Rpb[b], self.Rc, self.RA[13 + c]], writes=[self.RA[17 + c]])
        P.emit("pool", lambda e, o=self.halo[:, c * 30:(c + 1) * 30], i_=self.ut(c)[:, 512:542]: e.tensor_copy(out=o, in_=i_),
               reads=self.R_UT, writes=[self.Rhalo])

    def conv_stats(self):
        P = self.P
        b1, b2 = self.bank(), self.bank()
        for c in range(4):
            self.mm(self.pb[b1][:], self.ones_bf, self.ybf(c), c == 0, c == 3, [self.Rc, self.RA[13 + c]], [self.Rpb[b1]])
        for c in range(4):
            self.mm(self.pb[b2][:], self.ones_bf, self.ysq(c), c == 0, c == 3, [self.Rc, self.RA[17 + c]], [self.Rpb[b2]])
        M, T1 = self.Fs[0], self.Fs[1]
        P.emit("dve", lambda e: e.tensor_scalar(out=M[:], in0=self.pb[b1][:], scalar1=1.0 / 512, scalar2=None, op0=ALU.mult),
               reads=[self.Rpb[b1]], writes=[self.RF[0]])
        P.emit("dve", lambda e: e.tensor_tensor(out=T1[:], in0=M[:], in1=M[:], op=ALU.mult),
               reads=[self.RF[0]], writes=[self.RF[1]])
        P.emit("dve", lambda e: e.scalar_tensor_tensor(out=T1[:], in0=self.pb[b2][:], scalar=1.0 / 512, in1=T1[:],
                                                       op0=ALU.mult, op1=ALU.subtract),
               reads=[self.Rpb[b2], self.RF[1]], writes=[self.RF[1]])
        P.emit("dve", lambda e: e.tensor_scalar(out=T1[:], in0=T1[:], scalar1=0.0, scalar2=None, op0=ALU.max),
               reads=[self.RF[1]], writes=[self.RF[1]])

    def conv_rstd(self):
        P = self.P
        T1, RS = self.Fs[1], self.Fs[3]
        P.emit("act", lambda e: e.activation(out=RS[:], in_=T1[:], func=AF.Ln, bias=self.cst[:, 0:1]),
               reads=[self.RF[1], self.Rcst], writes=[self.RF[3]])
        P.emit("act", lambda e: e.activation(out=RS[:], in_=RS[:], func=AF.Exp, scale=-0.5),
               reads=[self.RF[3]], writes=[self.RF[3]])

    def conv_apply(self):
        P = self.P
        M, RS = self.Fs[0], self.Fs[3]
        for c in range(4):
            P.emit("dve", lambda e, y=self.ysq(c), i0=self.ybf(c): e.tensor_tensor(out=y, in0=i0, in1=M[:], op=ALU.subtract),
                   reads=[self.RA[13 + c], self.RF[0]], writes=[self.RA[17 + c]])
            P.emit("dve", lambda e, y=self.ysq(c): e.tensor_tensor(out=y, in0=y, in1=RS[:], op=ALU.mult),
                   reads=[self.RA[17 + c], self.RF[3]], writes=[self.RA[17 + c]])

    def conv_silu(self):
        P = self.P
        for c in range(4):
            P.emit("act", lambda e, o=self.hk(4 + c), y=self.ysq(c), sc=self.spc(C_LNG + c), bc=self.spc(C_LNB + c):
                   e.activation(out=o, in_=y, func=AF.Silu, scale=sc, bias=bc),
                   reads=[self.RA[17 + c], self.Rc], writes=[self.RH[4 + c]])
        self.preload_ln()

    def w_out(self, s, t):
        P = self.P
        self.norm_begin()
        pend = None
        for hf in range(2):
            sl, Rsl = self.wnext(("wout", hf))
            slv = sl[:].rearrange("p (m kc c) -> p m kc c", m=4, kc=8)
            for ml in range(4):
                mo = hf * 4 + ml
                b = self.bank()
                for kc in range(8):
                    self.mm(self.pb[b][:], slv[:, ml, kc, :], self.hk(kc), kc == 0, kc == 7,
                            [Rsl, self.RH[kc]], [self.Rpb[b]])
                P.emit("dve", lambda e, o=self.xk(mo), i0=self.pb[b][:]: e.tensor_tensor(out=o, in0=i0, in1=o, op=ALU.add),
                       reads=[self.Rpb[b], self.Rx[mo]], writes=[self.Rx[mo]])
                if pend is not None:
                    self.norm_chunk(pend)
                pend = mo
            self.wdone()
        self.norm_chunk(pend)

    def tile(self, s, t):
        P = self.P
        if s == 0 and t == 0:
            self.prefetch_x(0, 0)
        self.load_x(s, t)
        self.ffn(0)
        self.norm_finish()
        P.dma("pool", "qa", lambda e: e.dma_start(out=self.A[64:84, 0:4096], in_=self.qaux_d[t, :, :]),
              writes=self.RA[0:8])
        self.qk_chunks(s, t, "q")
        self.qk_chunks(s, t, "k")
        self.v_chunks(s, t)
        self.conv_in(s, t)
        self.kmean(s, t)
        self.conv_mm(0)
        self.conv_mm(1)
        self.gate_scores(s, t)
        self.conv_mm(2)
        self.conv_mm(3)
        self.gate_rows(s, t)
        self.attention(s, t, between={0: self.conv_stats, 1: self.conv_rstd, 2: self.conv_apply, 7: self.conv_silu})
        self.w_out(s, t)
        nxt = (s, t + 1) if t + 1 < self.NT else ((s + 1, 0) if s + 1 < self.NSEQ else None)
        self.ffn(1, after_up=(lambda: self.prefetch_x(*nxt)) if nxt else None)
        self.store_x(s, t)
        if s == 0 and t == 0:
            assert self.p0_cast == self.NSLAB
            vv = self.Vc[:].rearrange("p (c a s d) -> p c a s d", c=32, a=4, s=3, d=64)
            for c in range(10, 32):
                P.emit("pool", lambda e, o=vv[:, c, :, 1, :]: e.memset(o, 1.0), writes=[self.RV] + self.Rst)


def _bf(a):
    return np.asarray(a, dtype=np.float32).astype(ml_dtypes.bfloat16)


def make_consts():
    identf = np.eye(128, dtype=np.float32)
    cb = np.zeros((128, 512), np.float32)
    cb[:, 0:128] = np.eye(128)
    cb[:, 128:256] = 1.0
    cb[0:64, 256:320] = 1.0
    cb[64:128, 320:384] = 1.0
    kk = np.arange(128)[:, None]
    qq = np.arange(128)[None, :]
    cb[:, 384:512] = np.where(kk <= qq, 0.0, NEGB)
    kaux = np.zeros((20, 4096), np.float32)
    key = np.arange(4096)
    for n in range(16):
        kaux[n] = (key // 256 == n)
    kaux[16] = 1.0
    kaux[17] = 1.0
    kaux[18] = key % 256
    kaux[19] = 256 * (key // 256)
    slopes = 2.0 ** (-8.0 * (np.arange(8) + 1.0) / 8)
    qaux = np.zeros((8, 20, 8, 512), np.float32)
    it = np.arange(512)
    for t in range(8):
        bq = 2 * t + it // 256
        for n in range(16):
            qaux[t, n, :, :] = np.where(n <= bq, 0.0, NEGB)[None, :]
        for h in range(8):
            qaux[t, 16, h] = -slopes[h] * (it % 256)
            qaux[t, 17, h] = -slopes[h] * 256 * bq
            qaux[t, 18, h] = slopes[h]
            qaux[t, 19, h] = slopes[h]
    return identf, _bf(cb), _bf(kaux), _bf(qaux.reshape(8, 20, 4096))


def make_sp(inp):
    sp = np.zeros((128, NSP), np.float32)
    sp[:, C_G1:C_G1 + 8] = inp["ffn1_norm"][0].reshape(8, 128).T
    sp[:, C_GM:C_GM + 8] = inp["mix_norm"][0].reshape(8, 128).T
    sp[:, C_G2:C_G2 + 8] = inp["ffn2_norm"][0].reshape(8, 128).T
    sp[:, C_GQ] = np.tile(inp["q_norm"][0], 2)
    sp[:, C_GK] = np.tile(inp["k_norm"][0], 2)
    sp[:, C_DWB:C_DWB + 4] = inp["conv_dw_b"][0].reshape(4, 128).T
    sp[:, C_LNG:C_LNG + 4] = inp["conv_ln_g"][0].reshape(4, 128).T
    sp[:, C_LNB:C_LNB + 4] = inp["conv_ln_b"][0].reshape(4, 128).T
    w = inp["conv_dw_w"][0].reshape(31, 4, 128)
    sp[:, C_DWW:C_DWW + 124] = w.transpose(2, 1, 0).reshape(128, 124)
    return sp


_NC_CACHE = {}


def run(inp, n_cores=8, NSEQ=2, NT=8):
    key = (NSEQ, NT)
    if key not in _NC_CACHE:
        _NC_CACHE[key] = MK(NSEQ, NT).build()
    nc = _NC_CACHE[key]
    identf, cbf, kaux, qaux = make_consts()
    sp = make_sp(inp)
    f32 = lambda a: np.ascontiguousarray(a, dtype=np.float32)
    shared = {
        "f0w1": f32(inp["ffn1_w1"][0]), "f0w3": f32(inp["ffn1_w3"][0]), "f0w2": f32(inp["ffn1_w2"][0]),
        "f1w1": f32(inp["ffn2_w1"][0]), "f1w3": f32(inp["ffn2_w3"][0]), "f1w2": f32(inp["ffn2_w2"][0]),
        "win": f32(inp["w_in"][0]), "wout": f32(inp["w_out"][0]),
        "sp": sp, "identf": identf, "cbf": cbf, "kaux": kaux, "qaux": qaux,
    }
    S = NT * T
    x = inp["x"]
    in_maps = []
    for c in range(n_cores):
        m = dict(shared)
        m["x"] = f32(x[c * NSEQ:(c + 1) * NSEQ, :S, :])
        in_maps.append(m)
    res = run_bass_kernel_spmd(nc, in_maps, core_ids=list(range(n_cores)))
    return np.concatenate([r["out"] for r in res.results], axis=0)


def kernel(**inputs):
    inp = {k: np.asarray(v) for k, v in inputs.items()}
    out = run(inp, n_cores=8, NSEQ=2, NT=8)
    return out.astype(np.float32)
```

```python
import numpy as np
import ml_dtypes
from contextlib import ExitStack
import concourse.bass as bass
import concourse.mybir as mybir
from concourse.bass_utils import run_bass_kernel_spmd

F32 = mybir.dt.float32
BF16 = mybir.dt.bfloat16
AF = mybir.ActivationFunctionType
ALU = mybir.AluOpType
AX = mybir.AxisListType

D = 1024
DFF = 2816
NM = 22
T = 512
NEGB = -30000.0
EPS = 1e-6
NSLOT = 3
ENGS = ["pe", "act", "dve", "pool", "sp"]

C_G1, C_GM, C_G2, C_GQ, C_GK, C_DWB, C_LNG, C_LNB, C_DWW = 0, 8, 16, 24, 25, 26, 30, 34, 38
NSP = C_DWW + 124


class Res:
    __slots__ = ("name", "lw", "rd")

    def __init__(self, name):
        self.name = name
        self.lw = None
        self.rd = []


class Prog:
    def __init__(self, nc):
        self.nc = nc
        self.q = {e: [] for e in ENGS}
        self.tick = {e: 0 for e in ENGS}
        self.waited = {e: {} for e in ENGS}
        self.sems = {}
        self.dmacnt = {}

    def add_sem(self, name, handle):
        self.sems[name] = handle
        if name not in ENGS:
            self.dmacnt[name] = 0

    def _deps(self, eng, reads, writes):
        deps = {}

        def add(cv):
            if cv is None:
                return
            c, v = cv
            if deps.get(c, 0) < v:
                deps[c] = v
        for r in reads:
            add(r.lw)
        for w in writes:
            add(w.lw)
            for x in w.rd:
                add(x)
        out = []
        for c, v in deps.items():
            if c == eng and eng == "pe":
                continue
            if c in self.tick:
                assert v <= self.tick[c], ("wait on unsignalled tick", eng, c, v, self.tick[c])
            if self.waited[eng].get(c, 0) >= v:
                continue
            self.waited[eng][c] = v
            out.append((c, v))
        return out

    def _record(self, cv, reads, writes):
        for r in reads:
            r.rd.append(cv)
            if len(r.rd) > 64:
                m = {}
                for c, v in r.rd:
                    if m.get(c, 0) < v:
                        m[c] = v
                r.rd = list(m.items())
        for w in writes:
            w.lw = cv
            w.rd = []

    def emit(self, eng, fn, reads=(), writes=(), signal=True):
        waits = self._deps(eng, reads, writes)
        for c, v in waits:
            if c == eng:
                assert v <= self.tick[eng], (eng, v, self.tick[eng])
        if signal:
            self.tick[eng] += 1
            mytick = self.tick[eng]
        else:
            mytick = self.tick[eng] + 1
        sem = self.sems[eng]
        sems = self.sems

        def thunk(e):
            for c, v in waits:
                e.wait_ge(sems[c], v)
            ins = fn(e)
            if signal:
                ins.then_inc(sem, 1)
        self.q[eng].append(thunk)
        cv = (eng, mytick)
        self._record(cv, reads, writes)
        return cv

    def dma(self, eng, dsem, fn, reads=(), writes=()):
        waits = self._deps(eng, reads, writes)
        self.dmacnt[dsem] += 16
        val = self.dmacnt[dsem]
        sems = self.sems

        def thunk(e):
            for c, v in waits:
                e.wait_ge(sems[c], v)
            fn(e).then_inc(sems[dsem], 16)
        self.q[eng].append(thunk)
        cv = (dsem, val)
        self._record(cv, reads, writes)
        return cv

    def barrier(self):
        sems = self.sems
        for eng in ENGS:
            waits = []
            for c in ENGS:
                if c != eng and self.tick[c] > self.waited[eng].get(c, 0):
                    waits.append((c, self.tick[c]))
                    self.waited[eng][c] = self.tick[c]
            for c, v in self.dmacnt.items():
                if v > self.waited[eng].get(c, 0):
                    waits.append((c, v))
                    self.waited[eng][c] = v

            def thunk(e, waits=waits):
                for c, v in waits:
                    e.wait_ge(sems[c], v)
            self.q[eng].append(thunk)

    def wait_all(self, eng, ress):
        waits = self._deps(eng, (), ress)
        sems = self.sems

        def thunk(e):
            for c, v in waits:
                e.wait_ge(sems[c], v)
        self.q[eng].append(thunk)

    def run(self, block):
        q = self.q

        @block.tensor
        def _(e):
            for t in q["pe"]:
                t(e)

        @block.scalar
        def _(e):
            for t in q["act"]:
                t(e)

        @block.vector
        def _(e):
            for t in q["dve"]:
                t(e)

        @block.gpsimd
        def _(e):
            for t in q["pool"]:
                t(e)

        @block.sync
        def _(e):
            for t in q["sp"]:
                t(e)


def slab_descs():
    d = []
    for f in (0,):
        pass
    def ffn(f):
        for i in range(11):
            d.append(("up", f, i))
        for hf in range(2):
            for kg in range(3):
                d.append(("down", f, hf, kg))
    ffn(0)
    for nm in ("q", "k", "v", "za", "zg"):
        d.append(("win", nm))
    for c in range(4):
        d.append(("conv", c))
    for hf in range(2):
        d.append(("wout", hf))
    ffn(1)
    return d


class MK:
    def __init__(self, NSEQ, NT):
        self.NSEQ, self.NT = NSEQ, NT
        self.S = NT * T
        self.descs = slab_descs()
        self.NSLAB = len(self.descs)

    def build(self):
        nc = self.nc = bass.Bass("TRN2", target_bir_lowering=False)
        NSEQ, S = self.NSEQ, self.S
        dt = nc.dram_tensor
        self.x_d = dt("x", [NSEQ, S, D], F32, kind="ExternalInput").ap()
        self.w_d = {}
        for f in (0, 1):
            self.w_d[("w1", f)] = dt(f"f{f}w1", [D, DFF], F32, kind="ExternalInput").ap()
            self.w_d[("w3", f)] = dt(f"f{f}w3", [D, DFF], F32, kind="ExternalInput").ap()
            self.w_d[("w2", f)] = dt(f"f{f}w2", [DFF, D], F32, kind="ExternalInput").ap()
        self.win_d = dt("win", [D, 2560], F32, kind="ExternalInput").ap()
        self.wout_d = dt("wout", [D, D], F32, kind="ExternalInput").ap()
        self.sp_d = dt("sp", [128, NSP], F32, kind="ExternalInput").ap()
        self.idf_d = dt("identf", [128, 128], F32, kind="ExternalInput").ap()
        self.cbf_d = dt("cbf", [128, 512], BF16, kind="ExternalInput").ap()
        self.kaux_d = dt("kaux", [20, 4096], BF16, kind="ExternalInput").ap()
        self.qaux_d = dt("qaux", [8, 20, 4096], BF16, kind="ExternalInput").ap()
        self.out_d = dt("out", [NSEQ, S, D], F32, kind="ExternalOutput").ap()
        self.ws_d = dt("wstream", [self.NSLAB, 128, 4096], BF16).ap()

        with ExitStack() as es:
            def sb(name, shape, dtp):
                return es.enter_context(nc.sbuf_tensor(name, shape, dtp))

            def ps(name, shape, dtp):
                return es.enter_context(nc.psum_tensor(name, shape, dtp))
            self.xfm = sb("xfm", [128, 4096], F32)
            self.H = sb("H", [128, 2048], F32)
            self.A = sb("A", [128, NM * 512], BF16)
            self.Kc = [sb(f"Kc{h}", [84, 4096], BF16) for h in range(8)]
            self.Vc = sb("Vc", [128, 32 * 768], BF16)
            self.PT = sb("PT", [128, 6 * 512], BF16)
            self.Fall = sb("Fall", [128, 2048], F32)
            self.Fs = [self.Fall[:, i * 512:(i + 1) * 512] for i in range(4)]
            self.SQ = sb("SQ", [128, 2 * 512], BF16)
            self.ring = [sb(f"ring{i}", [128, 4096], BF16) for i in range(NSLOT)]
            self.spt = sb("spt", [128, NSP], F32)
            self.identf = sb("identf_sb", [128, 128], F32)
            self.cbf = sb("cbf_sb", [128, 512], BF16)
            self.cst = sb("cst", [128, 4], F32)
            self.kmT = sb("kmT", [64, 128], BF16)
            self.kmf = sb("kmf", [128, 2], F32)
            self.gsb = sb("gsb", [128, 128], F32)
            self.top8 = sb("top8", [128, 64], F32)
            self.msk = sb("msk", [128, 128], F32)
            self.mbw = sb("mbw", [128, 4 * 192], BF16)
            self.halo = sb("halo", [128, 4 * 30], BF16)
            self.pb = [ps(f"pb{i}", [128, 512], F32) for i in range(8)]

            P = self.P = Prog(nc)
            for e in ENGS:
                P.add_sem(e, es.enter_context(nc.semaphore("s_" + e)))
            dsems = ["cst", "ka", "qa", "st0", "st1"] + [f"xl{i}" for i in range(4)] + [f"xs{i}" for i in range(4)] + \
                [f"wl{i}" for i in range(NSLOT)] + [f"ws{i}" for i in range(NSLOT)]
            for dname in dsems:
                P.add_sem(dname, es.enter_context(nc.semaphore("d_" + dname)))
            block = es.enter_context(nc.Block())

            self.Rx = [Res(f"x{k}") for k in range(8)]
            self.RH = [Res(f"H{k}") for k in range(8)]
            self.RA = [Res(f"A{k}") for k in range(NM)]
            self.RK = [Res(f"K{h}") for h in range(8)]
            self.RV = Res("V")
            self.RPT = [Res(f"PT{i}") for i in range(6)]
            self.RF = [Res(f"F{i}") for i in range(4)]
            self.RSQ = [Res(f"SQ{i}") for i in range(2)]
            self.Rring = [Res(f"ring{i}") for i in range(NSLOT)]
            self.Rc = Res("consts")
            self.Rcst = Res("cst")
            self.Rcst2 = Res("cst2")
            self.Rcst3 = Res("cst3")
            self.Rkm = Res("kmT")
            self.Rkmf = Res("kmf")
            self.Rgsb = Res("gsb")
            self.Rtop = Res("top8")
            self.Rmsk = Res("msk")
            self.Rmbw = [Res(f"mbw{i}") for i in range(4)]
            self.Rpb = [Res(f"pb{i}") for i in range(8)]
            self.Rws = Res("wstream")
            self.Rslab = [Res(f"slab{i}") for i in range(self.NSLAB)]
            self.Rhalo = Res("halo")
            self.Rout = Res("out")
            self.rr = 0
            self.ptr = 0

            self.Hb = self.H[:].bitcast(BF16)
            self.ident_bf = self.cbf[:, 0:128]
            self.ones_bf = self.cbf[:, 128:256]
            self.bones_bf = self.cbf[:, 256:384]
            self.tri_bf = self.cbf[:, 384:512]

            self.phase0()
            self.init_main()
            self.wseq = [i for _ in range(NSEQ * self.NT) for i in range(self.NSLAB)]
            self.wpos = 0
            for s in range(NSEQ):
                self.seq_init(s)
                for t in range(self.NT):
                    self.tile(s, t)
            P.barrier()
            P.run(block)
        return nc

    def xk(self, k):
        return self.xfm[:, k * 512:(k + 1) * 512]

    def hk(self, k):
        return self.Hb[:, k * 512:(k + 1) * 512]

    def ak(self, m):
        return self.A[:, m * 512:(m + 1) * 512]

    def qa(self, h, rows=84):
        return self.A[0:rows, h * 512:(h + 1) * 512]

    def ut(self, c):
        return self.A[:, 4096 + c * 542: 4096 + (c + 1) * 542]

    R_UT = property(lambda self: self.RA[8:13])

    def ybf(self, c):
        return self.ak(13 + c)

    def ysq(self, c):
        return self.ak(17 + c)

    def bank(self):
        i = self.rr
        self.rr = (self.rr + 1) % 6
        return i

    def spc(self, c, lo=0, hi=128):
        return self.spt[lo:hi, c:c + 1]

    def eng_alt(self):
        self._alt = getattr(self, "_alt", 0) ^ 1
        return "act" if self._alt else "dve"

    def copy_any(self, eng, out, in_, reads, writes):
        if eng == "act":
            self.P.emit("act", lambda e: e.activation(out=out, in_=in_, func=AF.Copy), reads, writes)
        else:
            self.P.emit(eng, lambda e: e.tensor_copy(out=out, in_=in_), reads, writes)

    def scale_cast(self, eng, out, in_, col, reads, writes):
        if eng == "act":
            self.P.emit("act", lambda e: e.activation(out=out, in_=in_, func=AF.Copy, scale=col), reads, writes)
        else:
            self.P.emit("dve", lambda e: e.tensor_scalar(out=out, in0=in_, scalar1=col, scalar2=None, op0=ALU.mult),
                        reads, writes)

    def phase0(self):
        P = self.P
        P.dma("sp", "cst", lambda e: e.dma_start(out=self.spt[:], in_=self.sp_d[:, :]), writes=[self.Rc])
        P.dma("sp", "cst", lambda e: e.dma_start(out=self.identf[:], in_=self.idf_d[:, :]), writes=[self.Rc])
        P.dma("sp", "cst", lambda e: e.dma_start(out=self.cbf[:], in_=self.cbf_d[:, :]), writes=[self.Rc])
        vf = self.Vc[:].bitcast(F32)
        stg = [vf[:, 4096:8192], vf[:, 8192:12288]]
        Rst = self.Rst = [Res("stg0"), Res("stg1")]
        par, k = [], 0
        for desc in self.descs:
            par.append(k)
            if desc[0] != "conv":
                k ^= 1

        def loads(idx):
            desc = self.descs[idx]
            kk_ = par[idx]
            st, Rs, ds = stg[kk_], Rst[kk_], f"st{kk_}"
            kind = desc[0]
            if kind == "up":
                f, i = desc[1], desc[2]
                stv = st.rearrange("p (w kc n) -> p w kc n", w=2, kc=8, n=256)
                for w, nm in enumerate(("w1", "w3")):
                    src = self.w_d[(nm, f)].rearrange("(kc p) n -> p kc n", p=128)[:, :, 256 * i:256 * i + 256]
                    P.dma("sp", ds, lambda e, o=stv[:, w], s_=src: e.dma_start(out=o, in_=s_), writes=[Rs])
            elif kind == "down":
                f, hf, kg = desc[1], desc[2], desc[3]
                nk = min(8, NM - 8 * kg)
                src = self.w_d[("w2", f)].rearrange("(kc p) n -> p kc n", p=128)[:, 8 * kg:8 * kg + nk, 512 * hf:512 * hf + 512]
                stv = st[:, 0:nk * 512].rearrange("p (kc c) -> p kc c", kc=nk)
                P.dma("sp", ds, lambda e, o=stv, s_=src: e.dma_start(out=o, in_=s_), writes=[Rs])
            elif kind == "win":
                c0 = {"q": 0, "k": 512, "v": 1024, "za": 1536, "zg": 2048}[desc[1]]
                src = self.win_d.rearrange("(kc p) n -> p kc n", p=128)[:, :, c0:c0 + 512]
                stv = st.rearrange("p (kc n) -> p kc n", kc=8)
                P.dma("sp", ds, lambda e, o=stv, s_=src: e.dma_start(out=o, in_=s_), writes=[Rs])
            elif kind == "wout":
                hf = desc[1]
                src = self.wout_d.rearrange("(kc p) n -> p kc n", p=128)[:, :, hf * 512:hf * 512 + 512]
                stv = st.rearrange("p (kc n) -> p kc n", kc=8)
                P.dma("sp", ds, lambda e, o=stv, s_=src: e.dma_start(out=o, in_=s_), writes=[Rs])

        def casts(idx):
            desc = self.descs[idx]
            slot = idx % NSLOT
            sl = self.ring[slot][:]
            Rsl = self.Rring[slot]
            kk_ = par[idx]
            st, Rs = stg[kk_], Rst[kk_]
            kind = desc[0]
            if kind == "up":
                f, i = desc[1], desc[2]
                gc = C_G1 if f == 0 else C_G2
                stv = st.rearrange("p (w kc n) -> p w kc n", w=2, kc=8, n=256)
                slv = sl.rearrange("p (j w kc c) -> p j w kc c", j=2, w=2, kc=8, c=128)
                for w in range(2):
                    for kc in range(8):
                        self.scale_cast(self.eng_alt(), slv[:, :, w, kc, :],
                                        stv[:, w, kc, :].rearrange("p (j c) -> p j c", j=2),
                                        self.spc(gc + kc), [Rs, self.Rc], [Rsl])
            elif kind == "down":
                nk = min(8, NM - 8 * desc[3])
                self.copy_any(self.eng_alt(), sl[:, 0:nk * 512], st[:, 0:nk * 512], [Rs], [Rsl])
            elif kind == "win":
                nm = desc[1]
                stv = st.rearrange("p (kc n) -> p kc n", kc=8)
                for kc in range(8):
                    if nm == "v":
                        o = sl[:, kc * 512:(kc + 1) * 512]
                        i_ = stv[:, kc, :]
                    else:
                        o = sl.rearrange("p (m kc c) -> p m kc c", m=4, kc=8)[:, :, kc, :]
                        i_ = stv[:, kc, :].rearrange("p (m c) -> p m c", m=4)
                    self.scale_cast(self.eng_alt(), o, i_, self.spc(C_GM + kc), [Rs, self.Rc], [Rsl])
            elif kind == "conv":
                c = desc[1]
                for kk in range(31):
                    self.scale_cast(self.eng_alt(), sl[:, kk * 128:(kk + 1) * 128], self.identf[:],
                                    self.spc(C_DWW + c * 31 + kk), [self.Rc], [Rsl])
            elif kind == "wout":
                stv = st.rearrange("p (kc n) -> p kc n", kc=8)
                for kc in range(8):
                    o = sl.rearrange("p (m kc c) -> p m kc c", m=4, kc=8)[:, :, kc, :]
                    i_ = stv[:, kc, :].rearrange("p (m c) -> p m c", m=4)
                    self.copy_any(self.eng_alt(), o, i_, [Rs], [Rsl])
            P.dma("pool", f"ws{slot}", lambda e, o=self.ws_d[idx, :, :], s_=sl: e.dma_start(out=o, in_=s_),
                  reads=[Rsl], writes=[self.Rslab[idx]])

        self._p0_loads, self._p0_casts = loads, casts
        self.p0_loaded = 0
        self.p0_cast = 0

    def p0_advance(self, upto):
        n = self.NSLAB
        upto = min(upto, n)
        while self.p0_cast < upto:
            while self.p0_loaded < min(self.p0_cast + 2, n):
                self._p0_loads(self.p0_loaded)
                self.p0_loaded += 1
            self._p0_casts(self.p0_cast)
            self.p0_cast += 1

    def init_main(self):
        P = self.P
        vv = self.Vc[:].rearrange("p (c a s d) -> p c a s d", c=32, a=4, s=3, d=64)
        for c in range(10):
            P.emit("pool", lambda e, o=vv[:, c, :, 1, :]: e.memset(o, 1.0), writes=[self.RV])
        for h in range(8):
            P.dma("sp", "ka", lambda e, h=h: e.dma_start(out=self.Kc[h][64:84, :], in_=self.kaux_d[:, :]),
                  writes=[self.RK[h]])
        for h in range(8):
            self.RK[h].lw = ("ka", P.dmacnt["ka"])
        P.emit("pool", lambda e: e.memset(self.mbw[:], 0.0), writes=self.Rmbw)
        P.emit("dve", lambda e: e.memset(self.cst[:, 0:1], EPS), writes=[self.Rcst])
        P.emit("dve", lambda e: e.memset(self.cst[:, 2:3], 1.0), writes=[self.Rcst2])
        P.emit("dve", lambda e: e.tensor_scalar(out=self.cst[:, 1:2], in0=self.spc(C_GQ), scalar1=0.125,
                                                scalar2=None, op0=ALU.mult), reads=[self.Rc], writes=[self.Rcst])

    def seq_init(self, s):
        P = self.P
        P.emit("pool", lambda e: e.memset(self.halo[:], 0.0), writes=[self.Rhalo])
        P.emit("pool", lambda e: e.memset(self.gsb[:], NEGB), writes=[self.Rgsb])
        P.emit("pool", lambda e: e.memset(self.kmT[:], 0.0), writes=[self.Rkm])

    def _wload(self, j):
        if j < self.NSLAB:
            return
        slot = j % NSLOT
        idx = self.wseq[j]
        self.P.dma("sp", f"wl{slot}", lambda e: e.dma_start(out=self.ring[slot][:], in_=self.ws_d[idx, :, :]),
                   reads=[self.Rslab[idx]], writes=[self.Rring[slot]])

    def wnext(self, expect):
        j = self.wpos
        assert self.descs[self.wseq[j]] == expect, (self.descs[self.wseq[j]], expect)
        if j < self.NSLAB:
            self.p0_advance(j + NSLOT)
        slot = j % NSLOT
        return self.ring[slot], self.Rring[slot]

    def wdone(self):
        j = self.wpos
        self.wpos += 1
        if j + NSLOT < len(self.wseq):
            self._wload(j + NSLOT)

    def mm(self, out, lhsT, rhs, start, stop, reads, writes, signal=None):
        if signal is None:
            signal = stop
        self.P.emit("pe", lambda e: e.matmul(out, lhsT=lhsT, rhs=rhs, start=start, stop=stop),
                    reads, writes, signal)

    def stg_in(self, i):
        if i < 2:
            return self.H[:, i * 1024:(i + 1) * 1024], self.RH[4 * i:4 * i + 4]
        j = i - 2
        return self.Fall[:, j * 1024:(j + 1) * 1024], self.RF[2 * j:2 * j + 2]

    def stg_out(self, i):
        return self.A[:].bitcast(F32)[:, i * 1024:(i + 1) * 1024], self.RA[4 * i:4 * i + 4]

    def prefetch_x(self, s, t):
        for tc in range(4):
            st, Rst = self.stg_in(tc)
            r0 = t * T + tc * 128
            self.P.dma("pool", f"xl{tc}", lambda e, o=st, s_=self.x_d[s, r0:r0 + 128, :]: e.dma_start(out=o, in_=s_),
                       writes=Rst)

    def load_x(self, s, t):
        P = self.P
        self.norm_begin()
        self.preload_ln()
        pend = None
        for kc in range(8):
            b = self.bank()
            for tc in range(4):
                st, Rst = self.stg_in(tc)
                P.emit("pe", lambda e, o=self.pb[b][:, tc * 128:(tc + 1) * 128], i_=st[:, kc * 128:(kc + 1) * 128]:
                       e.transpose(out=o, in_=i_, identity=self.identf[:]),
                       reads=Rst + [self.Rc], writes=[self.Rpb[b]], signal=(tc == 3))
            self.copy_any("dve", self.xk(kc), self.pb[b][:], [self.Rpb[b]], [self.Rx[kc]])
            if pend is not None:
                self.norm_chunk(pend)
            pend = kc
        self.norm_chunk(pend)

    def store_x(self, s, t):
        P = self.P
        for tc in range(4):
            st, Rst = self.stg_out(tc)
            r0 = t * T + tc * 128
            for g in range(2):
                b = self.bank()
                for kl in range(4):
                    kc = g * 4 + kl
                    P.emit("pe", lambda e, o=self.pb[b][:, kl * 128:(kl + 1) * 128],
                           i_=self.xk(kc)[:, tc * 128:(tc + 1) * 128]:
                           e.transpose(out=o, in_=i_, identity=self.identf[:]),
                           reads=[self.Rx[kc], self.Rc], writes=[self.Rpb[b]], signal=(kl == 3))
                self.copy_any(self.eng_alt(), st[:, g * 512:(g + 1) * 512], self.pb[b][:], [self.Rpb[b]], Rst)
            P.dma("pool", f"xs{tc}", lambda e, o=self.out_d[s, r0:r0 + 128, :], s_=st: e.dma_start(out=o, in_=s_),
                  reads=Rst, writes=[self.Rout])

    def rstd_from(self, bnk, inv_n, Fi):
        P = self.P
        F, RF = self.Fs[Fi], self.RF[Fi]
        P.emit("act", lambda e: e.activation(out=F[:], in_=self.pb[bnk][:], func=AF.Ln, scale=inv_n,
                                             bias=self.cst[:, 0:1]),
               reads=[self.Rpb[bnk], self.Rcst], writes=[RF])
        P.emit("act", lambda e: e.activation(out=F[:], in_=F[:], func=AF.Exp, scale=-0.5), reads=[RF], writes=[RF])

    def preload_ln(self):
        self.P.emit("act", lambda e: e.activation(out=self.cst[:, 3:4], in_=self.cst[:, 2:3], func=AF.Ln),
                    reads=[self.Rcst2], writes=[self.Rcst3])

    def norm_begin(self):
        self.ncount = 0

    def norm_chunk(self, kc, from_bank=None):
        P = self.P
        i = self.ncount % 2
        if from_bank is None:
            src, Rsrc = self.xk(kc), self.Rx[kc]
        else:
            src, Rsrc = self.pb[from_bank][:], self.Rpb[from_bank]
        P.emit("act", lambda e, o=self.SQ[:, i * 512:(i + 1) * 512], i_=src: e.activation(out=o, in_=i_, func=AF.Square),
               reads=[Rsrc], writes=[self.RSQ[i]])
        self.mm(self.pb[6][:], self.ones_bf, self.SQ[:, i * 512:(i + 1) * 512], self.ncount == 0, self.ncount == 7,
                [self.RSQ[i], self.Rc], [self.Rpb[6]], signal=True)
        self.ncount += 1

    def norm_finish(self):
        P = self.P
        assert self.ncount == 8
        self.rstd_from(6, 1.0 / D, 3)
        for kc in range(8):
            eng = "pool" if kc in (2, 4, 6) else "dve"
            P.emit(eng, lambda e, o=self.hk(kc), i_=self.xk(kc): e.tensor_tensor(out=o, in0=i_, in1=self.Fs[3][:], op=ALU.mult),
                   reads=[self.Rx[kc], self.RF[3]], writes=[self.RH[kc]])

    def ffn(self, f, after_up=None):
        P = self.P
        self.norm_finish()
        for i in range(11):
            sl, Rsl = self.wnext(("up", f, i))
            slv = sl[:].rearrange("p (j w kc c) -> p j w kc c", j=2, w=2, kc=8, c=128)
            pre = None
            if i == 0:
                pre = [(self.bank(), self.bank()) for _ in range(2)]
                for kc in range(8):
                    for j in range(2):
                        for w in range(2):
                            bk = pre[j][w]
                            self.mm(self.pb[bk][:], slv[:, j, w, kc, :], self.hk(kc), kc == 0, kc == 7,
                                    [Rsl, self.RH[kc]], [self.Rpb[bk]])
            for j in range(2):
                m = 2 * i + j
                if pre is not None:
                    ba, bb = pre[j]
                else:
                    ba, bb = self.bank(), self.bank()
                    for w, bk in ((0, ba), (1, bb)):
                        for kc in range(8):
                            self.mm(self.pb[bk][:], slv[:, j, w, kc, :], self.hk(kc), kc == 0, kc == 7,
                                    [Rsl, self.RH[kc]], [self.Rpb[bk]])
                Fi = m % 2
                P.emit("act", lambda e, o=self.Fs[Fi][:], i_=self.pb[ba][:]: e.activation(out=o, in_=i_, func=AF.Silu),
                       reads=[self.Rpb[ba]], writes=[self.RF[Fi]])
                P.emit("dve", lambda e, o=self.ak(m), i0=self.pb[bb][:], i1=self.Fs[Fi][:]:
                       e.tensor_tensor(out=o, in0=i0, in1=i1, op=ALU.mult),
                       reads=[self.Rpb[bb], self.RF[Fi]], writes=[self.RA[m]])
            self.wdone()
        if after_up is not None:
            after_up()
        if f == 0:
            self.norm_begin()
            self.preload_ln()
        pend = []
        for hf in range(2):
            banks = [self.bank() for _ in range(4)]
            for kg in range(3):
                sl, Rsl = self.wnext(("down", f, hf, kg))
                nk = min(8, NM - 8 * kg)
                for ml in range(4):
                    bk = banks[ml]
                    for kl in range(nk):
                        kc = 8 * kg + kl
                        self.mm(self.pb[bk][:], sl[:, kl * 512 + ml * 128:kl * 512 + (ml + 1) * 128], self.ak(kc),
                                kc == 0, kc == NM - 1, [Rsl, self.RA[kc]], [self.Rpb[bk]],
                                signal=(kc == NM - 1) or (ml == 3 and kl == nk - 1))
                self.wdone()
            for ml in range(4):
                mo = hf * 4 + ml
                P.emit("dve", lambda e, o=self.xk(mo), i0=self.pb[banks[ml]][:]:
                       e.scalar_tensor_tensor(out=o, in0=i0, scalar=0.5, in1=o, op0=ALU.mult, op1=ALU.add),
                       reads=[self.Rpb[banks[ml]], self.Rx[mo]], writes=[self.Rx[mo]])
            if f == 0:
                for mo in pend:
                    self.norm_chunk(mo)
                pend = [hf * 4 + ml for ml in range(4)]
        if f == 0:
            for mo in pend:
                self.norm_chunk(mo)

    def qk_chunks(self, s, t, which):
        P = self.P
        sl, Rsl = self.wnext(("win", which))
        slv = sl[:].rearrange("p (m kc c) -> p m kc c", m=4, kc=8)
        col0 = t * T
        pend = None

        def finish(c, bz):
            i = c % 2
            bs = self.bank()
            self.mm(self.pb[bs][:], self.bones_bf, self.SQ[:, i * 512:(i + 1) * 512], True, True,
                    [self.RSQ[i], self.Rc], [self.Rpb[bs]])
            Fi = c % 2
            self.rstd_from(bs, 1.0 / 64, Fi)
            for hh in range(2):
                h = 2 * c + hh
                lo, hi = hh * 64, hh * 64 + 64
                if which == "q":
                    o, Ro = self.qa(h, 64), [self.RA[h]]
                    colap, Rcol = self.cst[lo:hi, 1:2], self.Rcst
                else:
                    o, Ro = self.Kc[h][0:64, col0:col0 + T], [self.RK[h]]
                    colap, Rcol = self.spc(C_GK, lo, hi), self.Rc
                P.emit("dve", lambda e, o=o, i0=self.pb[bz][lo:hi, :], i1=self.Fs[Fi][lo:hi, :], colap=colap:
                       e.scalar_tensor_tensor(out=o, in0=i0, scalar=colap, in1=i1, op0=ALU.mult, op1=ALU.mult),
                       reads=[self.Rpb[bz], self.RF[Fi], Rcol], writes=Ro)
        pre = {}
        if which == "q":
            pre = {0: self.bank(), 1: self.bank()}
            for kc in range(8):
                for c in (0, 1):
                    self.mm(self.pb[pre[c]][:], slv[:, c, kc, :], self.hk(kc), kc == 0, kc == 7,
                            [Rsl, self.RH[kc]], [self.Rpb[pre[c]]])
        for c in range(4):
            if c in pre:
                bz = pre[c]
            else:
                bz = self.bank()
                for kc in range(8):
                    self.mm(self.pb[bz][:], slv[:, c, kc, :], self.hk(kc), kc == 0, kc == 7,
                            [Rsl, self.RH[kc]], [self.Rpb[bz]])
            i = c % 2
            P.emit("act", lambda e, o=self.SQ[:, i * 512:(i + 1) * 512], i_=self.pb[bz][:]: e.activation(out=o, in_=i_, func=AF.Square),
                   reads=[self.Rpb[bz]], writes=[self.RSQ[i]])
            if pend is not None:
                finish(*pend)
            pend = (c, bz)
        self.wdone()
        finish(*pend)

    def v_chunks(self, s, t):
        sl, Rsl = self.wnext(("win", "v"))
        for tc in range(4):
            b = self.bank()
            for kc in range(8):
                self.mm(self.pb[b][:], self.hk(kc)[:, tc * 128:(tc + 1) * 128], sl[:, kc * 512:(kc + 1) * 512],
                        kc == 0, kc == 7, [Rsl, self.RH[kc]], [self.Rpb[b]])
            ch = t * 4 + tc
            o = self.Vc[:, ch * 768:(ch + 1) * 768].rearrange("p (a s d) -> p a s d", a=4, s=3, d=64)[:, :, 0:3:2, :]
            i_ = self.pb[b][:].rearrange("p (a s d) -> p a s d", a=4, s=2, d=64)
            self.copy_any(self.eng_alt(), o, i_, [self.Rpb[b]], [self.RV])
        self.wdone()

    def conv_in(self, s, t):
        P = self.P
        for c in range(4):
            P.emit("pool", lambda e, o=self.ut(c)[:, 0:30], i_=self.halo[:, c * 30:(c + 1) * 30]: e.tensor_copy(out=o, in_=i_),
                   reads=[self.Rhalo], writes=self.R_UT)
        sla, Rsla = self.wnext(("win", "za"))
        slav = sla[:].rearrange("p (m kc c) -> p m kc c", m=4, kc=8)
        banks = []
        for c in range(4):
            b = self.bank()
            for kc in range(8):
                self.mm(self.pb[b][:], slav[:, c, kc, :], self.hk(kc), kc == 0, kc == 7,
                        [Rsla, self.RH[kc]], [self.Rpb[b]])
            banks.append(b)
        self.wdone()
        slg, Rslg = self.wnext(("win", "zg"))
        slgv = slg[:].rearrange("p (m kc c) -> p m kc c", m=4, kc=8)
        for c in range(4):
            b = 6 + (c % 2)
            for kc in range(8):
                self.mm(self.pb[b][:], slgv[:, c, kc, :], self.hk(kc), kc == 0, kc == 7,
                        [Rslg, self.RH[kc]], [self.Rpb[b]])
            Fi = c % 2
            P.emit("act", lambda e, o=self.Fs[Fi][:], i_=self.pb[b][:]: e.activation(out=o, in_=i_, func=AF.Sigmoid),
                   reads=[self.Rpb[b]], writes=[self.RF[Fi]])
            P.emit("dve", lambda e, o=self.ut(c)[:, 30:542], i0=self.pb[banks[c]][:], i1=self.Fs[Fi][:]:
                   e.tensor_tensor(out=o, in0=i0, in1=i1, op=ALU.mult),
                   reads=[self.Rpb[banks[c]], self.RF[Fi]], writes=self.R_UT)
        self.wdone()

    def kmean(self, s, t):
        P = self.P
        col0 = t * T
        for h in range(8):
            kin = self.Kc[h][0:64, col0:col0 + T].rearrange("p (b k) -> p b k", b=2)
            P.emit("dve", lambda e, i_=kin: e.tensor_reduce(out=self.kmf[0:64, :], in_=i_, axis=AX.X, op=ALU.add),
                   reads=[self.RK[h]], writes=[self.Rkmf])
            P.emit("dve", lambda e, o=self.kmT[0:64, h * 16 + 2 * t:h * 16 + 2 * t + 2]:
                   e.tensor_scalar(out=o, in0=self.kmf[0:64, :], scalar1=1.0 / 256, scalar2=None, op0=ALU.mult),
                   reads=[self.Rkmf], writes=[self.Rkm])

    def gate_scores(self, s, t):
        P = self.P
        if t < 2:
            return
        for qc in range(4):
            b = 2 * t + qc // 2
            g = self.bank()
            for h in range(8):
                self.mm(self.pb[g][:, h * 16:(h + 1) * 16], self.qa(h, 64)[:, qc * 128:(qc + 1) * 128],
                        self.kmT[0:64, h * 16:(h + 1) * 16], True, True,
                        [self.RA[h], self.Rkm], [self.Rpb[g]], signal=(h == 7))
            gv = self.gsb[:].rearrange("p (h n) -> p h n", h=8)
            P.emit("dve", lambda e, o=gv[:, :, 0:b], i_=self.pb[g][:, 0:128].rearrange("p (h n) -> p h n", h=8)[:, :, 0:b]:
                   e.tensor_copy(out=o, in_=i_), reads=[self.Rpb[g]], writes=[self.Rgsb])
            for h in range(8):
                P.emit("dve", lambda e, o=self.top8[:, h * 8:(h + 1) * 8], i_=self.gsb[:, h * 16:(h + 1) * 16]:
                       e.max(out=o, in_=i_), reads=[self.Rgsb], writes=[self.Rtop])
            thr = self.top8[:].rearrange("p (h k) -> p h k", h=8)[:, :, 2:3].broadcast_to([128, 8, 16])
            P.emit("dve", lambda e, thr=thr: e.tensor_tensor(out=self.msk[:].rearrange("p (h n) -> p h n", h=8), in0=gv, in1=thr,
                                                            op=ALU.is_ge),
                   reads=[self.Rgsb, self.Rtop], writes=[self.Rmsk])
            mo = self.mbw[:, qc * 192 + 64:qc * 192 + 192]
            P.emit("dve", lambda e, mo=mo: e.tensor_scalar(out=mo, in0=self.msk[:], scalar1=1.0, scalar2=-NEGB,
                                                          op0=ALU.subtract, op1=ALU.mult),
                   reads=[self.Rmsk], writes=[self.Rmbw[qc]])
            P.emit("dve", lambda e, o=mo.rearrange("p (h n) -> p h n", h=8)[:, :, b:b + 1]: e.memset(o, 0.0),
                   writes=[self.Rmbw[qc]])

    def gate_rows(self, s, t):
        P = self.P
        if t < 2:
            return
        for h in range(8):
            g = self.bank()
            gb = self.pb[g][:].bitcast(BF16)
            for qc in range(4):
                P.emit("pe", lambda e, o=gb[0:80, qc * 128:(qc + 1) * 128],
                       i_=self.mbw[:, qc * 192 + 16 * h:qc * 192 + 16 * h + 80]:
                       e.transpose(out=o, in_=i_, identity=self.ident_bf),
                       reads=[self.Rmbw[qc], self.Rc], writes=[self.Rpb[g]], signal=(qc == 3))
            self.copy_any("act", self.qa(h)[64:80, :], gb[64:80, 0:512], [self.Rpb[g]], [self.RA[h]])

    def attention(self, s, t, between=None):
        P = self.P
        b0, b1 = 2 * t, 2 * t + 1
        for h in range(8):
            acc = 6 + (h % 2)
            pair, odd = h // 2, h % 2
            vlo = pair * 192 + (64 if odd else 0)
            qh = self.qa(h)
            chunks = []
            for n in range(b0):
                for kc in range(2):
                    chunks.append((n * 256 + kc * 128, 0, 512, False))
            chunks.append((b0 * 256, 0, 512, True))
            chunks.append((b0 * 256 + 128, 128, 512, True))
            chunks.append((b1 * 256, 256, 512, True))
            chunks.append((b1 * 256 + 128, 384, 512, True))
            staged = []

            def qk(ci):
                k0, ql, qhi, tri = chunks[ci]
                n_ = qhi - ql
                sb_ = self.bank()
                self.mm(self.pb[sb_][:, 0:n_], self.Kc[h][0:84, k0:k0 + 128], qh[:, ql:qhi],
                        True, not tri, [self.RK[h], self.RA[h]], [self.Rpb[sb_]])
                if tri:
                    self.mm(self.pb[sb_][:, 0:128], self.ident_bf, self.tri_bf, False, True,
                            [self.Rc], [self.Rpb[sb_]])
                pi = self.ptr
                self.ptr = (self.ptr + 1) % 6
                P.emit("act", lambda e, o=self.PT[:, pi * 512:pi * 512 + n_], i_=self.pb[sb_][:, 0:n_]:
                       e.activation(out=o, in_=i_, func=AF.Exp),
                       reads=[self.Rpb[sb_]], writes=[self.RPT[pi]])
                staged.append((ci, pi))

            def pv(first, last):
                ci, pi = staged.pop(0)
                k0, ql, qhi, tri = chunks[ci]
                n_ = qhi - ql
                ch = k0 // 128
                self.mm(self.pb[acc][:, ql:qhi], self.Vc[:, ch * 768 + vlo:ch * 768 + vlo + 128],
                        self.PT[:, pi * 512:pi * 512 + n_], first, last,
                        [self.RV, self.RPT[pi]], [self.Rpb[acc]], signal=(last or ci + LOOK >= nch))
            nch = len(chunks)
            LOOK = 4
            for ci in range(min(LOOK, nch)):
                qk(ci)
            for ci in range(nch):
                pv(ci == 0, ci == nch - 1)
                if ci + LOOK < nch:
                    qk(ci + LOOK)
            alo, dlo = (64, 0) if odd else (0, 64)
            rec = self.Fs[2][alo:alo + 64, :]
            if h == 7:
                P.emit("act", lambda e, rec=rec, i_=self.pb[acc][dlo:dlo + 64, :]: e.activation(out=rec, in_=i_, func=AF.Ln),
                       reads=[self.Rpb[acc]], writes=[self.RF[2]])
                P.emit("act", lambda e, rec=rec: e.activation(out=rec, in_=rec, func=AF.Exp, scale=-1.0),
                       reads=[self.RF[2]], writes=[self.RF[2]])
            else:
                P.emit("dve", lambda e, rec=rec, i_=self.pb[acc][dlo:dlo + 64, :]: e.reciprocal(out=rec, in_=i_),
                       reads=[self.Rpb[acc]], writes=[self.RF[2]])
            o = self.hk(pair)[alo:alo + 64, :]
            P.emit("dve", lambda e, o=o, i0=self.pb[acc][alo:alo + 64, :], rec=rec:
                   e.tensor_tensor(out=o, in0=i0, in1=rec, op=ALU.mult),
                   reads=[self.Rpb[acc], self.RF[2]], writes=[self.RH[pair]])
            if between and h in between:
                between[h]()

    def conv_mm(self, c):
        P = self.P
        sl, Rsl = self.wnext(("conv", c))
        b = self.bank()
        for kk in range(31):
            self.mm(self.pb[b][:], sl[:, kk * 128:(kk + 1) * 128], self.ut(c)[:, kk:kk + 512], kk == 0, kk == 30,
                    [Rsl] + self.R_UT, [self.Rpb[b]])
        self.wdone()
        P.emit("act", lambda e, o=self.ybf(c), i_=self.pb[b][:], bc=self.spc(C_DWB + c):
               e.activation(out=o, in_=i_, func=AF.Identity, bias=bc),
               reads=[self.Rpb[b], self.Rc], writes=[self.RA[13 + c]])
        P.emit("act", lambda e, o=self.ysq(c), i_=self.pb[b][:], bc=self.spc(C_DWB + c):
               e.activation(out=o, in_=i_, func=AF.Square, bias=bc),
               reads=[self.Rpb[b], self.Rc], writes=[self.RA[17 + c]])
        P.emit("pool", lambda e, o=self.halo[:, c * 30:(c + 1) * 30], i_=self.ut(c)[:, 512:542]: e.tensor_copy(out=o, in_=i_),
               reads=self.R_UT, writes=[self.Rhalo])

    def conv_stats(self):
        P = self.P
        b1, b2 = self.bank(), self.bank()
        for c in range(4):
            self.mm(self.pb[b1][:], self.ones_bf, self.ybf(c), c == 0, c == 3, [self.Rc, self.RA[13 + c]], [self.Rpb[b1]])
        for c in range(4):
            self.mm(self.pb[b2][:], self.ones_bf, self.ysq(c), c == 0, c == 3, [self.Rc, self.RA[17 + c]], [self.Rpb[b2]])
        M, T1 = self.Fs[0], self.Fs[1]
        P.emit("dve", lambda e: e.tensor_scalar(out=M[:], in0=self.pb[b1][:], scalar1=1.0 / 512, scalar2=None, op0=ALU.mult),
               reads=[self.Rpb[b1]], writes=[self.RF[0]])
        P.emit("dve", lambda e: e.tensor_tensor(out=T1[:], in0=M[:], in1=M[:], op=ALU.mult),
               reads=[self.RF[0]], writes=[self.RF[1]])
        P.emit("dve", lambda e: e.scalar_tensor_tensor(out=T1[:], in0=self.pb[b2][:], scalar=1.0 / 512, in1=T1[:],
                                                       op0=ALU.mult, op1=ALU.subtract),
               reads=[self.Rpb[b2], self.RF[1]], writes=[self.RF[1]])
        P.emit("dve", lambda e: e.tensor_scalar(out=T1[:], in0=T1[:], scalar1=0.0, scalar2=None, op0=ALU.max),
               reads=[self.RF[1]], writes=[self.RF[1]])

    def conv_rstd(self):
        P = self.P
        T1, RS = self.Fs[1], self.Fs[3]
        P.emit("act", lambda e: e.activation(out=RS[:], in_=T1[:], func=AF.Ln, bias=self.cst[:, 0:1]),
               reads=[self.RF[1], self.Rcst], writes=[self.RF[3]])
        P.emit("act", lambda e: e.activation(out=RS[:], in_=RS[:], func=AF.Exp, scale=-0.5),
               reads=[self.RF[3]], writes=[self.RF[3]])

    def conv_apply(self):
        P = self.P
        M, RS = self.Fs[0], self.Fs[3]
        for c in range(4):
            P.emit("dve", lambda e, y=self.ysq(c), i0=self.ybf(c): e.tensor_tensor(out=y, in0=i0, in1=M[:], op=ALU.subtract),
                   reads=[self.RA[13 + c], self.RF[0]], writes=[self.RA[17 + c]])
            P.emit("dve", lambda e, y=self.ysq(c): e.tensor_tensor(out=y, in0=y, in1=RS[:], op=ALU.mult),
                   reads=[self.RA[17 + c], self.RF[3]], writes=[self.RA[17 + c]])

    def conv_silu(self):
        P = self.P
        for c in range(4):
            P.emit("act", lambda e, o=self.hk(4 + c), y=self.ysq(c), sc=self.spc(C_LNG + c), bc=self.spc(C_LNB + c):
                   e.activation(out=o, in_=y, func=AF.Silu, scale=sc, bias=bc),
                   reads=[self.RA[17 + c], self.Rc], writes=[self.RH[4 + c]])
        self.preload_ln()

    def w_out(self, s, t):
        P = self.P
        self.norm_begin()
        pend = None
        for hf in range(2):
            sl, Rsl = self.wnext(("wout", hf))
            slv = sl[:].rearrange("p (m kc c) -> p m kc c", m=4, kc=8)
            for ml in range(4):
                mo = hf * 4 + ml
                b = self.bank()
                for kc in range(8):
                    self.mm(self.pb[b][:], slv[:, ml, kc, :], self.hk(kc), kc == 0, kc == 7,
                            [Rsl, self.RH[kc]], [self.Rpb[b]])
                P.emit("dve", lambda e, o=self.xk(mo), i0=self.pb[b][:]: e.tensor_tensor(out=o, in0=i0, in1=o, op=ALU.add),
                       reads=[self.Rpb[b], self.Rx[mo]], writes=[self.Rx[mo]])
                if pend is not None:
                    self.norm_chunk(pend)
                pend = mo
            self.wdone()
        self.norm_chunk(pend)

    def tile(self, s, t):
        P = self.P
        if s == 0 and t == 0:
            self.prefetch_x(0, 0)
        self.load_x(s, t)
        self.ffn(0)
        self.norm_finish()
        P.dma("pool", "qa", lambda e: e.dma_start(out=self.A[64:84, 0:4096], in_=self.qaux_d[t, :, :]),
              writes=self.RA[0:8])
        self.qk_chunks(s, t, "q")
        self.qk_chunks(s, t, "k")
        self.v_chunks(s, t)
        self.conv_in(s, t)
        self.kmean(s, t)
        self.conv_mm(0)
        self.conv_mm(1)
        self.gate_scores(s, t)
        self.conv_mm(2)
        self.conv_mm(3)
        self.gate_rows(s, t)
        self.attention(s, t, between={0: self.conv_stats, 1: self.conv_rstd, 2: self.conv_apply, 7: self.conv_silu})
        self.w_out(s, t)
        nxt = (s, t + 1) if t + 1 < self.NT else ((s + 1, 0) if s + 1 < self.NSEQ else None)
        self.ffn(1, after_up=(lambda: self.prefetch_x(*nxt)) if nxt else None)
        self.store_x(s, t)
        if s == 0 and t == 0:
            assert self.p0_cast == self.NSLAB
            vv = self.Vc[:].rearrange("p (c a s d) -> p c a s d", c=32, a=4, s=3, d=64)
            for c in range(10, 32):
                P.emit("pool", lambda e, o=vv[:, c, :, 1, :]: e.memset(o, 1.0), writes=[self.RV] + self.Rst)


def _bf(a):
    return np.asarray(a, dtype=np.float32).astype(ml_dtypes.bfloat16)


def make_consts():
    identf = np.eye(128, dtype=np.float32)
    cb = np.zeros((128, 512), np.float32)
    cb[:, 0:128] = np.eye(128)
    cb[:, 128:256] = 1.0
    cb[0:64, 256:320] = 1.0
    cb[64:128, 320:384] = 1.0
    kk = np.arange(128)[:, None]
    qq = np.arange(128)[None, :]
    cb[:, 384:512] = np.where(kk <= qq, 0.0, NEGB)
    kaux = np.zeros((20, 4096), np.float32)
    key = np.arange(4096)
    for n in range(16):
        kaux[n] = (key // 256 == n)
    kaux[16] = 1.0
    kaux[17] = 1.0
    kaux[18] = key % 256
    kaux[19] = 256 * (key // 256)
    slopes = 2.0 ** (-8.0 * (np.arange(8) + 1.0) / 8)
    qaux = np.zeros((8, 20, 8, 512), np.float32)
    it = np.arange(512)
    for t in range(8):
        bq = 2 * t + it // 256
        for n in range(16):
            qaux[t, n, :, :] = np.where(n <= bq, 0.0, NEGB)[None, :]
        for h in range(8):
            qaux[t, 16, h] = -slopes[h] * (it % 256)
            qaux[t, 17, h] = -slopes[h] * 256 * bq
            qaux[t, 18, h] = slopes[h]
            qaux[t, 19, h] = slopes[h]
    return identf, _bf(cb), _bf(kaux), _bf(qaux.reshape(8, 20, 4096))


def make_sp(inp):
    sp = np.zeros((128, NSP), np.float32)
    sp[:, C_G1:C_G1 + 8] = inp["ffn1_norm"][0].reshape(8, 128).T
    sp[:, C_GM:C_GM + 8] = inp["mix_norm"][0].reshape(8, 128).T
    sp[:, C_G2:C_G2 + 8] = inp["ffn2_norm"][0].reshape(8, 128).T
    sp[:, C_GQ] = np.tile(inp["q_norm"][0], 2)
    sp[:, C_GK] = np.tile(inp["k_norm"][0], 2)
    sp[:, C_DWB:C_DWB + 4] = inp["conv_dw_b"][0].reshape(4, 128).T
    sp[:, C_LNG:C_LNG + 4] = inp["conv_ln_g"][0].reshape(4, 128).T
    sp[:, C_LNB:C_LNB + 4] = inp["conv_ln_b"][0].reshape(4, 128).T
    w = inp["conv_dw_w"][0].reshape(31, 4, 128)
    sp[:, C_DWW:C_DWW + 124] = w.transpose(2, 1, 0).reshape(128, 124)
    return sp


_NC_CACHE = {}


def run(inp, n_cores=8, NSEQ=2, NT=8):
    key = (NSEQ, NT)
    if key not in _NC_CACHE:
        _NC_CACHE[key] = MK(NSEQ, NT).build()
    nc = _NC_CACHE[key]
    identf, cbf, kaux, qaux = make_consts()
    sp = make_sp(inp)
    f32 = lambda a: np.ascontiguousarray(a, dtype=np.float32)
    shared = {
        "f0w1": f32(inp["ffn1_w1"][0]), "f0w3": f32(inp["ffn1_w3"][0]), "f0w2": f32(inp["ffn1_w2"][0]),
        "f1w1": f32(inp["ffn2_w1"][0]), "f1w3": f32(inp["ffn2_w3"][0]), "f1w2": f32(inp["ffn2_w2"][0]),
        "win": f32(inp["w_in"][0]), "wout": f32(inp["w_out"][0]),
        "sp": sp, "identf": identf, "cbf": cbf, "kaux": kaux, "qaux": qaux,
    }
    S = NT * T
    x = inp["x"]
    in_maps = []
    for c in range(n_cores):
        m = dict(shared)
        m["x"] = f32(x[c * NSEQ:(c + 1) * NSEQ, :S, :])
        in_maps.append(m)
    res = run_bass_kernel_spmd(nc, in_maps, core_ids=list(range(n_cores)))
    return np.concatenate([r["out"] for r in res.results], axis=0)


def kernel(**inputs):
    inp = {k: np.asarray(v) for k, v in inputs.items()}
    out = run(inp, n_cores=8, NSEQ=2, NT=8)
    return out.astype(np.float32)
```
